# Optimizing a Trainium2 kernel written in Bass

```python
import math
import jax
import jax.numpy as jnp
from jax import lax
import numpy as np

D_MODEL = 1024
BATCH = 4
SEQ = 8192
DEPTH = 4
DEC_BATCH = 8
DEC_SEQ = 32
PAST_LEN = 1024

CHUNK = 64
N_MIXERS = 3
HEAD_DIM = 64
N_HEADS = D_MODEL // HEAD_DIM
N_KV_HEADS = 4
GROUP = N_HEADS // N_KV_HEADS
WINDOW = 128
BAND_CHUNKS = WINDOW // CHUNK
NUM_BUCKETS = 32
MAX_DISTANCE = 128
RWKV_HEAD = 64
RWKV_HEADS = D_MODEL // RWKV_HEAD
D_DECAY_LORA = 64
D_AAA_LORA = 64
D_GATE_LORA = 128
GN_EPS = RWKV_HEAD * 1e-5
CONV_WIDTH = 3
D_FF = 4 * D_MODEL
RMS_EPS = 1e-6

N_A = len(range(0, DEPTH, N_MIXERS))
N_B = len(range(1, DEPTH, N_MIXERS))
N_C = len(range(2, DEPTH, N_MIXERS))

kernel_name = "hybrid_swa_rwkv7_shortconv_stream_step"

F32 = jnp.float32


def _rmsnorm(x, g):
    xf = x.astype(F32)
    y = xf * lax.rsqrt(jnp.mean(xf * xf, axis=-1, keepdims=True) + RMS_EPS)
    return (y * g.astype(F32)).astype(x.dtype)


def _rel_bucket(rp):
    nb = NUM_BUCKETS // 2
    ret = (rp > 0).astype(jnp.int32) * nb
    n = jnp.abs(rp)
    max_exact = nb // 2
    nf = jnp.maximum(n, 1).astype(F32)
    large = max_exact + (jnp.log(nf / max_exact) / math.log(MAX_DISTANCE / max_exact)
                         * (nb - max_exact)).astype(jnp.int32)
    large = jnp.minimum(large, nb - 1)
    return ret + jnp.where(n < max_exact, n, large)


def _rel_bias(table, q_len, k_len, k_offset):
    i = jnp.arange(q_len)[:, None]
    j = jnp.arange(k_len)[None, :]
    b = table[_rel_bucket(j - k_offset - i)]
    return jnp.transpose(b, (2, 0, 1)).reshape(N_KV_HEADS, GROUP, q_len, k_len)


def _attn_core(q, k, v, bias, mask, sinks):
    s = jnp.einsum('bnqhgd,bnkhd->bnhgqk', q, k).astype(F32) * (HEAD_DIM ** -0.5) + bias.astype(F32)
    if mask is not None:
        s = jnp.where(mask[None, :, None, None], s, -jnp.inf)
    sink = sinks.astype(F32).reshape(1, 1, N_KV_HEADS, GROUP, 1, 1)
    m = jnp.maximum(jnp.max(s, axis=-1, keepdims=True), sink)
    p = jnp.exp(s - m)
    den = jnp.sum(p, axis=-1, keepdims=True) + jnp.exp(sink - m)
    return jnp.einsum('bnhgqk,bnkhd->bnqhgd', (p / den).astype(v.dtype), v)


def _qkv(h, w):
    B, T, _ = h.shape
    qkv = h @ w
    nq, nk = N_HEADS * HEAD_DIM, N_KV_HEADS * HEAD_DIM
    q = qkv[..., :nq].reshape(B, T, N_HEADS, HEAD_DIM)
    k = qkv[..., nq:nq + nk].reshape(B, T, N_KV_HEADS, HEAD_DIM)
    v = qkv[..., nq + nk:].reshape(B, T, N_KV_HEADS, HEAD_DIM)
    return q, k, v


def _attn_prompt(h, P, j):
    B, T, _ = h.shape
    nc = T // CHUNK
    q, k, v = _qkv(h, P['a_w_qkv'][j])
    q = q.reshape(B, nc, CHUNK, N_KV_HEADS, GROUP, HEAD_DIM)

    def band(t):
        tc = t.reshape(B, nc, CHUNK, N_KV_HEADS, HEAD_DIM)
        tp = jnp.pad(tc, ((0, 0), (BAND_CHUNKS, 0), (0, 0), (0, 0), (0, 0)))
        return jnp.concatenate([tp[:, s:s + nc] for s in range(BAND_CHUNKS + 1)], axis=2)

    kl = (BAND_CHUNKS + 1) * CHUNK
    key_chunk = jnp.arange(nc)[:, None, None] - BAND_CHUNKS + (jnp.arange(kl) // CHUNK)[None, None, :]
    mask = key_chunk >= 0
    bias = _rel_bias(P['rel_bias_table'], CHUNK, kl, BAND_CHUNKS * CHUNK)
    o = _attn_core(q, band(k), band(v), bias, mask, P['a_sinks'][j])
    y = o.reshape(B, T, N_HEADS * HEAD_DIM) @ P['a_w_o'][j]
    return y, k[:, -WINDOW:], v[:, -WINDOW:]


def _attn_sample(h, ck, cv, P, j):
    B, S, _ = h.shape
    W = ck.shape[1]
    q, k, v = _qkv(h, P['a_w_qkv'][j])
    q = q.reshape(B, 1, S, N_KV_HEADS, GROUP, HEAD_DIM)
    kf = jnp.concatenate([ck.astype(k.dtype), k], axis=1)
    vf = jnp.concatenate([cv.astype(v.dtype), v], axis=1)
    bias = _rel_bias(P['rel_bias_table'], S, W + S, W)
    o = _attn_core(q, kf[:, None], vf[:, None], bias, None, P['a_sinks'][j])
    y = o.reshape(B, S, N_HEADS * HEAD_DIM) @ P['a_w_o'][j]
    return y, kf[:, -W:], vf[:, -W:]


def _rwkv7(x, shift0, S0, P, j):
    B, T, D = x.shape
    mu = P['b_mu'][j]
    xx = jnp.concatenate([shift0[:, None].astype(x.dtype), x[:, :-1]], axis=1) - x
    xr, xw, xk, xv, xa, xg = [x + xx * mu[i] for i in range(6)]
    w_rkv = P['b_w_rkv'][j]
    r = (xr @ w_rkv[0]).astype(F32)
    k = (xk @ w_rkv[1]).astype(F32)
    v = (xv @ w_rkv[2]).astype(F32)
    w = -jax.nn.softplus(-(P['b_w0'][j] + jnp.tanh(xw @ P['b_w1'][j]) @ P['b_w2'][j]).astype(F32)) - 0.5
    decay = jnp.exp(-jnp.exp(w))
    a = jax.nn.sigmoid((P['b_a0'][j] + (xa @ P['b_a1'][j]) @ P['b_a2'][j]).astype(F32))
    g = jax.nn.sigmoid(xg @ P['b_g1'][j]) @ P['b_g2'][j]
    heads = lambda t: t.reshape(B, T, RWKV_HEADS, RWKV_HEAD)
    kk = heads(k * P['b_k_k'][j].astype(F32))
    kk = kk * lax.rsqrt(jnp.maximum(jnp.sum(kk * kk, axis=-1, keepdims=True), 1e-24))
    k = k * (1.0 + (a - 1.0) * P['b_k_a'][j].astype(F32))
    r_h, k_h, v_h, a_h, d_h = heads(r), heads(k), heads(v), heads(a), heads(decay)

    def step(S, inp):
        r_t, k_t, v_t, kk_t, a_t, d_t = inp
        sa = jnp.einsum('bhvk,bhk->bhv', S, -kk_t)
        S = S * d_t[:, :, None, :] + sa[..., None] * (kk_t * a_t)[:, :, None, :] + v_t[..., None] * k_t[:, :, None, :]
        return S, jnp.einsum('bhvk,bhk->bhv', S, r_t)

    xs = tuple(jnp.moveaxis(t, 1, 0) for t in (r_h, k_h, v_h, kk, a_h, d_h))
    S_fin, y = lax.scan(step, S0.astype(F32), xs)
    y = jnp.moveaxis(y, 0, 1)
    mean = jnp.mean(y, axis=-1, keepdims=True)
    var = jnp.mean(jnp.square(y - mean), axis=-1, keepdims=True)
    yn = ((y - mean) * lax.rsqrt(var + GN_EPS)).reshape(B, T, D) * P['b_ln_w'][j].astype(F32) + P['b_ln_b'][j].astype(F32)
    bonus = (jnp.sum(r_h * k_h * P['b_r_k'][j].astype(F32), axis=-1, keepdims=True) * v_h).reshape(B, T, D)
    out = ((yn + bonus).astype(x.dtype) * g) @ P['b_w_o'][j]
    return out, S_fin, x[:, -1]


def _short_conv(x, prev_u, P, j):
    T = x.shape[1]
    bg, cg, hh = jnp.split(x @ P['c_w_in'][j], 3, axis=-1)
    u = cg * hh
    up = jnp.concatenate([prev_u.astype(u.dtype), u], axis=1)
    cw = P['c_conv_w'][j]
    y = sum(up[:, t:t + T] * cw[t] for t in range(CONV_WIDTH))
    return (bg * y) @ P['c_w_out'][j], up[:, -(CONV_WIDTH - 1):]


def _sqrelu_mlp(x, w1, w2):
    h = jax.nn.relu(x @ w1)
    return (h * h) @ w2


def _trunk(x, prompt, a_k, a_v, b_wkv, b_shift, c_conv, P):
    B = x.shape[0]
    nk, nv, nwkv, nsh, ncv = [], [], [], [], []
    g = P['norm_g']
    for i in range(DEPTH):
        kind, j = i % N_MIXERS, i // N_MIXERS
        h = _rmsnorm(x, g[i, 0])
        if kind == 0:
            if prompt:
                m, k_new, v_new = _attn_prompt(h, P, j)
            else:
                m, k_new, v_new = _attn_sample(h, a_k[j], a_v[j], P, j)
            nk.append(k_new)
            nv.append(v_new)
        elif kind == 1:
            if prompt:
                sh0 = jnp.zeros((B, D_MODEL), x.dtype)
                s0 = jnp.zeros((B, RWKV_HEADS, RWKV_HEAD, RWKV_HEAD), F32)
            else:
                sh0, s0 = b_shift[j], b_wkv[j]
            m, s_new, sh_new = _rwkv7(h, sh0, s0, P, j)
            nwkv.append(s_new)
            nsh.append(sh_new)
        else:
            u0 = jnp.zeros((B, CONV_WIDTH - 1, D_MODEL), x.dtype) if prompt else c_conv[j]
            m, u_new = _short_conv(h, u0, P, j)
            ncv.append(u_new)
        x = x + _rmsnorm(m, g[i, 1])
        x = x + _rmsnorm(_sqrelu_mlp(_rmsnorm(x, g[i, 2]), P['mlp_w1'][i], P['mlp_w2'][i]), g[i, 3])
    return x, jnp.stack(nk), jnp.stack(nv), jnp.stack(nwkv), jnp.stack(nsh), jnp.stack(ncv)


def setup_inputs(seed: int = 0) -> dict:
    key = jax.random.key(seed)
    ks = iter(jax.random.split(key, 48))
    nrm = lambda shape, scale: scale * jax.random.normal(next(ks), shape, F32)
    D = D_MODEL
    win_rows = min(WINDOW, PAST_LEN)
    d_qkv = (N_HEADS + 2 * N_KV_HEADS) * HEAD_DIM
    return {
        'x_prompt': nrm((BATCH, SEQ, D), 1.0),
        'x_sample': nrm((DEC_BATCH, DEC_SEQ, D), 1.0),
        'cache_a_k': nrm((N_A, DEC_BATCH, win_rows, N_KV_HEADS, HEAD_DIM), 1.0),
        'cache_a_v': nrm((N_A, DEC_BATCH, win_rows, N_KV_HEADS, HEAD_DIM), 1.0),
        'state_b_wkv': nrm((N_B, DEC_BATCH, RWKV_HEADS, RWKV_HEAD, RWKV_HEAD), 0.5),
        'state_b_shift': nrm((N_B, DEC_BATCH, D), 1.0),
        'state_c_conv': nrm((N_C, DEC_BATCH, CONV_WIDTH - 1, D), 1.0),
        'rel_bias_table': nrm((NUM_BUCKETS, N_HEADS), 0.5),
        'norm_g': 1.0 + nrm((DEPTH, 4, D), 0.05),
        'a_w_qkv': nrm((N_A, D, d_qkv), D ** -0.5),
        'a_w_o': nrm((N_A, N_HEADS * HEAD_DIM, D), (N_HEADS * HEAD_DIM) ** -0.5),
        'a_sinks': nrm((N_A, N_HEADS), 1.0),
        'b_mu': jax.random.uniform(next(ks), (N_B, 6, D), F32),
        'b_w_rkv': nrm((N_B, 3, D, D), D ** -0.5),
        'b_w_o': nrm((N_B, D, D), D ** -0.5),
        'b_w0': nrm((N_B, D), 0.5) - 1.0,
        'b_w1': nrm((N_B, D, D_DECAY_LORA), D ** -0.5),
        'b_w2': nrm((N_B, D_DECAY_LORA, D), 0.5 * D_DECAY_LORA ** -0.5),
        'b_a0': nrm((N_B, D), 0.1),
        'b_a1': nrm((N_B, D, D_AAA_LORA), D ** -0.5),
        'b_a2': nrm((N_B, D_AAA_LORA, D), 0.5 * D_AAA_LORA ** -0.5),
        'b_g1': nrm((N_B, D, D_GATE_LORA), D ** -0.5),
        'b_g2': nrm((N_B, D_GATE_LORA, D), D_GATE_LORA ** -0.5),
        'b_k_k': 0.85 + nrm((N_B, D), 0.05),
        'b_k_a': 1.0 + nrm((N_B, D), 0.05),
        'b_r_k': nrm((N_B, RWKV_HEADS, RWKV_HEAD), 0.1),
        'b_ln_w': 1.0 + nrm((N_B, D), 0.05),
        'b_ln_b': nrm((N_B, D), 0.02),
        'c_w_in': nrm((N_C, D, 3 * D), D ** -0.5),
        'c_conv_w': nrm((N_C, CONV_WIDTH, D), CONV_WIDTH ** -0.5),
        'c_w_out': nrm((N_C, D, D), D ** -0.5),
        'mlp_w1': nrm((DEPTH, D, D_FF), D ** -0.5),
        'mlp_w2': nrm((DEPTH, D_FF, D), D_FF ** -0.5),
    }


def reference(x_prompt, x_sample, cache_a_k, cache_a_v, state_b_wkv, state_b_shift, state_c_conv,
              rel_bias_table, norm_g, a_w_qkv, a_w_o, a_sinks,
              b_mu, b_w_rkv, b_w_o, b_w0, b_w1, b_w2, b_a0, b_a1, b_a2, b_g1, b_g2,
              b_k_k, b_k_a, b_r_k, b_ln_w, b_ln_b,
              c_w_in, c_conv_w, c_w_out, mlp_w1, mlp_w2):
    P = dict(rel_bias_table=rel_bias_table, norm_g=norm_g, a_w_qkv=a_w_qkv, a_w_o=a_w_o, a_sinks=a_sinks,
             b_mu=b_mu, b_w_rkv=b_w_rkv, b_w_o=b_w_o, b_w0=b_w0, b_w1=b_w1, b_w2=b_w2,
             b_a0=b_a0, b_a1=b_a1, b_a2=b_a2, b_g1=b_g1, b_g2=b_g2, b_k_k=b_k_k, b_k_a=b_k_a,
             b_r_k=b_r_k, b_ln_w=b_ln_w, b_ln_b=b_ln_b, c_w_in=c_w_in, c_conv_w=c_conv_w,
             c_w_out=c_w_out, mlp_w1=mlp_w1, mlp_w2=mlp_w2)
    y_prompt, ak_p, av_p, wkv_p, sh_p, cv_p = _trunk(x_prompt, True, None, None, None, None, None, P)
    y_sample, ak_s, av_s, wkv_s, sh_s, cv_s = _trunk(x_sample, False, cache_a_k, cache_a_v,
                                                     state_b_wkv, state_b_shift, state_c_conv, P)
    return (y_prompt, y_sample, ak_p, av_p, ak_s, av_s, wkv_p, wkv_s, sh_p, sh_s, cv_p, cv_s)
```

```python
from contextlib import ExitStack
import math
import numpy as np
import concourse.bass as bass
import concourse.mybir as mybir
from concourse.bass_utils import run_bass_kernel_spmd

F32 = mybir.dt.float32
BF16 = mybir.dt.bfloat16
AF = mybir.ActivationFunctionType
ALU = mybir.AluOpType

D = 1024
NH = 16
DH = 64
NKV = 4
DFF = 4096
WIN = 128
DEC_SEQ = 32
NEG = -30000.0
C0 = math.exp(-0.5)
NVEC = 32
V_MU, V_W0, V_A0, V_KK, V_KA, V_RK, V_LNW, V_LNB, V_CW = 16, 22, 23, 24, 25, 26, 27, 28, 29
K_J, K_MP, K_MC, K_ONES, K_BD, K_M1, K_ML, K_SEG, K_OH, K_EPS, K_GEPS, K_ZERO, K_I2, K_NC = (
    0, 384, 512, 640, 768, 896, 1024, 1088, 1600, 1984, 1985, 1986, 1988, 2052)
NSLOT = 5
LOOKAHEAD = 2
SLOT_ELEMS = 4096


class Res:
    __slots__ = ("name", "w", "rs", "excl")

    def __init__(self, name, excl=False):
        self.name = name
        self.w = None
        self.rs = []
        self.excl = excl


class Sched:
    ENG = ("pe", "act", "dve", "pool", "sp")

    def __init__(self, sems, record=False):
        self.record = record
        self.ops = {e: [] for e in self.ENG}
        self.sem = dict(sems)
        self.cnt = {e: 0 for e in self.ENG}
        self.seen = {e: {} for e in self.ENG}
        self.chan_cnt = {}

    def _need(self, eng, dep, same_ok):
        if dep is None:
            return
        key, val = dep
        if key == eng and same_ok:
            return
        if self.seen[eng].get(key, 0) >= val:
            return
        self.seen[eng][key] = val
        sem = self.sem[key]
        self.ops[eng].append(lambda e, sem=sem, val=val: e.wait_ge(sem, val))

    def _deps(self, eng, reads, writes):
        for r in reads:
            self._need(eng, r.w, same_ok=(eng == "pe"))
            if r.excl:
                for rd in r.rs:
                    self._need(eng, rd, same_ok=True)
        for w in writes:
            self._need(eng, w.w, same_ok=True)
            for rd in w.rs:
                self._need(eng, rd, same_ok=True)

    def _mark(self, tag, reads, writes):
        for r in reads:
            r.rs.append(tag)
            if len(r.rs) > 16:
                mx = {}
                for k, v in r.rs:
                    mx[k] = max(mx.get(k, 0), v)
                r.rs = list(mx.items())
        for w in writes:
            w.w = tag
            w.rs = []

    def op(self, eng, fn, reads=(), writes=()):
        if self.record:
            return
        self._deps(eng, reads, writes)
        self.cnt[eng] += 1
        sem = self.sem[eng]
        self.ops[eng].append(lambda e, fn=fn, sem=sem: fn(e).then_inc(sem, 1))
        self._mark((eng, self.cnt[eng]), reads, writes)

    def group(self, eng, fns, reads=(), writes=()):
        if self.record:
            return
        self._deps(eng, reads, writes)
        self.cnt[eng] += 1
        sem = self.sem[eng]
        for fn in fns[:-1]:
            self.ops[eng].append(fn)
        fn = fns[-1]
        self.ops[eng].append(lambda e, fn=fn, sem=sem: fn(e).then_inc(sem, 1))
        self._mark((eng, self.cnt[eng]), reads, writes)

    def dma(self, q, chan, pairs, reads=(), writes=(), slow=False):
        if self.record:
            return
        self._deps(q, reads, writes)
        n = self.chan_cnt.get(chan, 0)
        if n:
            self._need(q, (chan, n), same_ok=False)
        sem = self.sem[chan]
        for (o, i) in pairs:
            if slow:
                self.ops[q].append(lambda e, o=o, i=i, sem=sem: e.dma_start(
                    out=o, in_=i, allow_slow_non_contiguous=True).then_inc(sem, 16))
            else:
                self.ops[q].append(lambda e, o=o, i=i, sem=sem: e.dma_start(out=o, in_=i).then_inc(sem, 16))
        n += 16 * len(pairs)
        self.chan_cnt[chan] = n
        self._mark((chan, n), reads, writes)

    def barrier(self, engines=("pe", "act", "dve", "pool", "sp")):
        if self.record:
            return
        for e in engines:
            for k in self.ENG:
                if k != e and self.cnt[k]:
                    self._need(e, (k, self.cnt[k]), same_ok=False)
            for ch, n in self.chan_cnt.items():
                if not ch.startswith("w"):
                    self._need(e, (ch, n), same_ok=False)

    def finish(self, q="sp"):
        for chan, n in self.chan_cnt.items():
            self._need(q, (chan, n), same_ok=False)
        for e in self.ENG:
            if e != q and self.cnt[e]:
                self._need(q, (e, self.cnt[e]), same_ok=False)

    def replay(self, block):
        ops = self.ops

        @block.tensor
        def _(e):
            for f in ops["pe"]:
                f(e)

        @block.scalar
        def _(e):
            for f in ops["act"]:
                f(e)

        @block.vector
        def _(e):
            for f in ops["dve"]:
                f(e)

        @block.gpsimd
        def _(e):
            for f in ops["pool"]:
                f(e)

        @block.sync
        def _(e):
            for f in ops["sp"]:
                f(e)


def MM(out, lhsT, rhs, start, stop):
    return lambda e: e.matmul(out, lhsT, rhs, start=start, stop=stop)


def TR(out, in_, ident):
    return lambda e: e.transpose(out, in_, ident)


def _rel_bucket_np(rp):
    nb = 16
    ret = (rp > 0).astype(np.int64) * nb
    n = np.abs(rp)
    max_exact = nb // 2
    nf = np.maximum(n, 1).astype(np.float32)
    large = max_exact + (np.log(nf / np.float32(max_exact)) / np.float32(math.log(128 / max_exact))
                         * np.float32(nb - max_exact)).astype(np.int64)
    large = np.minimum(large, nb - 1)
    return ret + np.where(n < max_exact, n, large)


def make_consts():
    c = np.zeros((128, K_NC), np.float32)
    p = np.arange(128)
    for i in range(128):
        c[i, K_J + 128 + i] = 1.0
    k = p[:, None]
    q = p[None, :]
    c[:, K_MP:K_MP + 128] = np.where((k < 64) & (q >= 64), NEG, 0.0)
    c[:, K_MC:K_MC + 128] = np.where((k >= 64) & (q < 64), NEG, 0.0)
    c[:, K_ONES:K_ONES + 128] = 1.0
    c[:, K_BD:K_BD + 128] = ((k // 64) == (q // 64)).astype(np.float32)
    s = (p % 64)[:, None]
    t = np.arange(64)[None, :]
    c[:, K_M1:K_M1 + 64] = (s < t).astype(np.float32)
    c[:, K_M1 + 64:K_M1 + 128] = (s <= t).astype(np.float32)
    c[:, K_ML:K_ML + 64] = (t < s).astype(np.float32)
    seg = np.ones(512, np.float32)
    seg[::64] = 0.0
    c[:, K_SEG:K_SEG + 512] = seg[None, :]
    r = np.arange(384)
    b = _rel_bucket_np(r - 255)
    for i in range(384):
        c[b[i], K_OH + i] = 1.0
    c[:, K_EPS] = 1e-6
    c[:, K_GEPS] = 64 * 1e-5
    c[:, K_ZERO] = 0.0
    c[:, K_ZERO + 1] = 1e-24
    c[:, K_I2:K_I2 + 64] = (s == t).astype(np.float32)
    return c


def build_program(NPRE, NMAIN, full_prefix=False):
    import os
    ATT = int(os.environ.get('KATT', '9'))
    NL = float(os.environ.get('KLAYERS', '9'))
    DBG = set(os.environ.get('KDEBUG', 'attn,mlp,rwkv,conv,sample,bias').split(','))
    nc = bass.Bass("TRN2", target_bir_lowering=False)
    NTOKP = (NPRE + NMAIN) * 512

    def din(name, shape):
        return nc.dram_tensor(name, list(shape), F32, kind="ExternalInput").ap()

    def dout(name, shape):
        return nc.dram_tensor(name, list(shape), F32, kind="ExternalOutput").ap()

    I = dict(
        xp=din("xp", [NTOKP, D]), xs=din("xs", [DEC_SEQ, D]),
        ck=din("ck", [2, 128, 256]), cv=din("cv", [2, 128, 256]),
        h0=din("h0", [128, 8 * 64]), sh0=din("sh0", [128, 8]), cv0=din("cv0", [128, 16]),
        pmask=din("pmask", [128, 1]), consts=din("consts", [128, K_NC]), cols=din("cols", [128, NVEC * 8]),
        table=din("table", [32, 16]), sinks=din("sinks", [1, 32]),
        a_w_qkv=din("a_w_qkv", [2, D, 1536]), a_w_o=din("a_w_o", [2, D, D]),
        b_w_rkv=din("b_w_rkv", [3, D, D]), b_w_o=din("b_w_o", [D, D]),
        b_w1=din("b_w1", [D, 64]), b_w2=din("b_w2", [64, D]), b_a1=din("b_a1", [D, 64]), b_a2=din("b_a2", [64, D]),
        b_g1=din("b_g1", [D, 128]), b_g2=din("b_g2", [128, D]),
        c_w_in=din("c_w_in", [D, 3 * D]), c_w_out=din("c_w_out", [D, D]),
        mlp_w1=din("mlp_w1", [4, D, DFF]), mlp_w2=din("mlp_w2", [4, DFF, D]),
    )
    O = dict(
        yp=dout("yp", [NMAIN * 512, D]), ys=dout("ys", [DEC_SEQ, D]),
        akp=dout("akp", [2, 128, 256]), avp=dout("avp", [2, 128, 256]),
        aks=dout("aks", [2, 128, 256]), avs=dout("avs", [2, 128, 256]),
        wkvp=dout("wkvp", [128, 512]), wkvs=dout("wkvs", [128, 512]),
        shp=dout("shp", [128, 8]), shs=dout("shs", [128, 8]),
        cvp=dout("cvp", [128, 16]), cvs=dout("cvs", [128, 16]),
    )

    BIGW = ["a_w_qkv", "a_w_o", "b_w_rkv", "b_w_o", "c_w_in", "c_w_out", "mlp_w1", "mlp_w2"]
    WB = {n: nc.dram_tensor(n + "_bf", list(I[n].shape), BF16).ap() for n in BIGW}

    with ExitStack() as es:
        def sb(name, shape, dt=F32):
            return es.enter_context(nc.sbuf_tensor(name, list(shape), dt))

        xT = sb("xT", [128, 8 * 512])
        rstd = sb("rstd", [128, 512])
        tmpA = [sb("tmpA0", [128, 512]), sb("tmpA1", [128, 512])]
        slots = [sb(f"slot{i}", [128, SLOT_ELEMS], BF16) for i in range(NSLOT)]
        biasT = [sb("biasP", [128, 16 * 128], BF16), sb("biasC", [128, 16 * 128], BF16)]
        cst = sb("cst", [128, K_NC])
        colv = sb("colv", [128, NVEC * 8])
        omk = sb("omk", [128, 8])
        lw1 = sb("lw1", [128, 8 * 256], BF16)
        lw2 = sb("lw2", [64, 2 * 1024], BF16)
        lg2 = sb("lg2", [128, 1024], BF16)
        ones_bf = sb("ones_bf", [128, 128], BF16)
        bd_bf = sb("bd_bf", [128, 128], BF16)
        stage0_ = sb("stage0", [128, 1024])
        stage = [stage0_, stage0_]
        es_t = sb("es_t", [128, 32])
        tab = sb("tab", [32, 16])
        fsb = sb("fsb", [128, 3 * 16])
        pmk = sb("pmk", [128, 1])
        kvst = sb("kvst", [128, 512])
        pflag = sb("pflag", [128, 1])
        kcar = [[sb(f"kcar{s}{l}", [64, 4 * 128], BF16) for l in range(2)] for s in range(2)]
        vcar = [[sb(f"vcar{s}{l}", [128, 256], BF16) for l in range(2)] for s in range(2)]
        Hst = [sb(f"H{s}", [128, 8 * 64]) for s in range(2)]
        shc = [sb(f"shc{s}", [128, 8]) for s in range(2)]
        ucar = [sb(f"ucar{s}", [128, 16]) for s in range(2)]
        ARENA_F32 = (nc.sbuf_bytes_remaining - 1024) // 4
        arena = sb("arena", [128, ARENA_F32])
        psb = [es.enter_context(nc.psum_tensor(f"psb{i}", [128, 512], F32)) for i in range(8)]

        chans = ([f"ws{i}" for i in range(NSLOT)] + ["xin0", "yout0", "yout1", "min", "minp"] + [f"wc{i}" for i in range(4)])
        sems = {n: es.enter_context(nc.semaphore("s_" + n)) for n in list(Sched.ENG) + chans}
        block = es.enter_context(nc.Block())

        def cs(off, n=1):
            return cst[:, off:off + n]

        ident = cst[:, K_J + 128:K_J + 256]

        def col(v, c):
            return colv[:, v * 8 + c:v * 8 + c + 1]

        def emit(S):
            R = {}

            def res(name):
                if name not in R:
                    R[name] = Res(name)
                return R[name]

            ps_rr = [0]
            RPS = [res(f"ps{i}") for i in range(8)]
            for r_ in RPS:
                r_.excl = True

            def bank():
                i = ps_rr[0] % 7
                ps_rr[0] += 1
                return psb[i], RPS[i]

            class Arena:
                def __init__(self):
                    self.off = 0

                def reset(self):
                    self.off = 0

                def f32(self, n):
                    v = arena[:, self.off:self.off + n]
                    self.off += n
                    assert self.off <= ARENA_F32, ("arena overflow", self.off, ARENA_F32)
                    return v

                def bf16(self, n):
                    assert n % 2 == 0
                    return self.f32(n // 2).bitcast(BF16)

            AR = Arena()
            r_arena_gen = [0]

            def ares(name):
                return res(f"ar{r_arena_gen[0]}_{name}")

            def phase(sp=False):
                S.barrier(("pe", "act", "dve", "pool", "sp") if sp else ("pe", "act", "dve", "pool"))
                AR.reset()
                r_arena_gen[0] += 1

            wstate = {"i": 0, "issued": 0}
            RSLOT = [res(f"slot{i}") for i in range(NSLOT)]

            def wget(dram3d, shape3):
                i = wstate["i"]
                wstate["i"] += 1
                if S.record:
                    wsched.append((dram3d, shape3))
                    return None, None
                while wstate["issued"] < min(len(wsched), i + LOOKAHEAD + 1):
                    n = wstate["issued"]
                    d3, sh = wsched[n]
                    sl = n % NSLOT
                    view = slots[sl][0:sh[0], 0:sh[1] * sh[2]].rearrange("p (a b) -> p a b", a=sh[1])
                    if not S.record:
                        for dep in conv_done:
                            S._need("sp", dep, False)
                    S.dma("sp", f"ws{sl}", [(view, d3)], writes=[RSLOT[sl]])
                    wstate["issued"] += 1
                sl = i % NSLOT
                view = slots[sl][0:shape3[0], 0:shape3[1] * shape3[2]].rearrange("p (a b) -> p a b", a=shape3[1])
                return view, RSLOT[sl]

            def wslab_k(W2d, c0, ncols, kc=8):
                return wget(W2d.rearrange("(k p) o -> p k o", p=128)[:, :, c0:c0 + ncols], (128, kc, ncols))

            conv_done = []
            if not S.record:
                ci = 0
                for n_ in BIGW:
                    src, dst = I[n_], WB[n_]
                    mats = [(src, dst)] if len(src.shape) == 2 else [(src[i_], dst[i_]) for i_ in range(src.shape[0])]
                    for (s2, d2) in mats:
                        rows, cols_ = s2.shape
                        step = 128 if cols_ >= 3072 else 256
                        for r0 in range(0, rows, step):
                            S.dma("pool", f"wc{ci % 4}", [(d2[r0:r0 + step, :], s2[r0:r0 + step, :])])
                            ci += 1
                conv_done = [(f"wc{i_}", S.chan_cnt[f"wc{i_}"]) for i_ in range(4)]
            Rc = res("cst")
            S.dma("sp", "min", [(cst[:, :], I["consts"][:, :])], writes=[Rc])
            Rcol = res("colv")
            S.dma("sp", "min", [(colv[:, :], I["cols"][:, :])], writes=[Rcol])
            Rtab = res("tab")
            S.dma("sp", "min", [(tab[:, :], I["table"][:, :])], writes=[Rtab])
            Res_es = res("es")
            S.dma("sp", "min", [(es_t[:, :], I["sinks"][0:1, :].broadcast_to([128, 32]))], writes=[Res_es])
            Rpm = res("pmk")
            S.dma("sp", "min", [(pmk[:, :], I["pmask"][:, :])], writes=[Rpm])
            Rlw = res("lw")
            S.dma("pool", "minp", [
                (lw1[:, :].rearrange("p (k o) -> p k o", k=8)[:, :, 0:64], I["b_w1"].rearrange("(k p) o -> p k o", p=128)),
                (lw1[:, :].rearrange("p (k o) -> p k o", k=8)[:, :, 64:128], I["b_a1"].rearrange("(k p) o -> p k o", p=128)),
                (lw1[:, :].rearrange("p (k o) -> p k o", k=8)[:, :, 128:256], I["b_g1"].rearrange("(k p) o -> p k o", p=128)),
                (lw2[:, 0:1024], I["b_w2"][:, :]), (lw2[:, 1024:2048], I["b_a2"][:, :]), (lg2[:, :], I["b_g2"][:, :]),
            ], writes=[Rlw])
            S.op("dve", lambda e: e.tensor_scalar(pflag[:, :], pmk[:, :], -1.0 / NEG, 1.0, ALU.mult, ALU.add),
                 reads=[Rpm], writes=[res("pflag")])
            S.op("dve", lambda e: e.tensor_copy(ones_bf[:, :], cs(K_ONES, 128)), reads=[Rc], writes=[res("ones_bf")])
            S.op("dve", lambda e: e.tensor_copy(bd_bf[:, :], cs(K_BD, 128)), reads=[Rc], writes=[res("bd_bf")])
            S.op("dve", lambda e: e.tensor_scalar(omk[:, :], colv[:, V_KA * 8:V_KA * 8 + 8], -1.0, 1.0, ALU.mult, ALU.add),
                 reads=[Rcol], writes=[res("omk")])
            S.op("act", lambda e: e.activation(out=es_t[:, :], in_=es_t[:, :], func=AF.Exp), reads=[Res_es], writes=[Res_es])
            Rf = res("fsb")
            for rc in range(3):
                pb, rp_ = bank()
                S.group("pe", [MM(pb[:, 0:16], cst[0:32, K_OH + rc * 128:K_OH + (rc + 1) * 128], tab[:, :], True, True)],
                        reads=[Rc, Rtab], writes=[rp_])
                S.op("act", lambda e, pb=pb, rc=rc: e.activation(out=fsb[:, rc * 16:(rc + 1) * 16], in_=pb[:, 0:16], func=AF.Copy),
                     reads=[rp_], writes=[Rf])
            Rb = [res("biasP"), res("biasC")]
            for kb in (range(2) if 'bias' in DBG else []):
                bt3 = biasT[kb][:, :].rearrange("p (h q) -> p h q", h=16)
                for q in range(128):
                    off = (127 - q) if kb == 0 else (255 - q)
                    rc0 = off // 128
                    pb, rp_ = bank()
                    fns = []
                    for j, rc in enumerate((rc0, rc0 + 1)):
                        s_ = off - rc * 128
                        fns.append(MM(pb[:, 0:16], cst[:, K_J + s_ + 128:K_J + s_ + 256], fsb[:, rc * 16:(rc + 1) * 16],
                                      j == 0, j == 1))
                    S.group("pe", fns, reads=[Rc, Rf], writes=[rp_])
                    mcol = (K_MP if kb == 0 else K_MC) + q
                    S.op("dve", lambda e, pb=pb, bt3=bt3, q=q, mcol=mcol: e.tensor_scalar(
                        bt3[:, :, q], pb[:, 0:16], cs(mcol), None, ALU.add), reads=[rp_, Rc], writes=[Rb[kb]])

            Rx = res("xT")
            Rrstd = res("rstd")
            Rtmp = [res("tmpA0"), res("tmpA1")]
            tmp_rr = [0]

            def x3(NT):
                return xT[:, :].rearrange("p (c t) -> p c t", c=8)[:, :, 0:NT]

            def rms_stats(src_c, Rsrc, NT, sq, Rsq):
                for c in range(8):
                    S.op("act", lambda e, c=c: e.activation(out=sq[:, c, 0:NT], in_=src_c(c), func=AF.Square),
                         reads=[Rsrc], writes=[Rsq])
                pb, rp_ = bank()
                S.group("pe", [MM(pb[:, 0:NT], ones_bf[:, :], sq[:, c, 0:NT], c == 0, c == 7) for c in range(8)],
                        reads=[Rsq, res("ones_bf")], writes=[rp_])
                S.op("act", lambda e, pb=pb: e.activation(out=rstd[:, 0:NT], in_=pb[:, 0:NT], func=AF.Sqrt,
                                                         bias=cs(K_EPS), scale=1.0 / D), reads=[rp_, Rc], writes=[Rrstd])
                S.op("dve", lambda e: e.reciprocal(rstd[:, 0:NT], rstd[:, 0:NT]), reads=[Rrstd], writes=[Rrstd])

            def norm_apply(gi, dst_c, Rdst, NT):
                xv = x3(NT)
                for c in range(8):
                    S.op("dve", lambda e, c=c: e.scalar_tensor_tensor(dst_c(c), xv[:, c, :], col(gi, c), rstd[:, 0:NT],
                                                                      ALU.mult, ALU.mult),
                         reads=[Rx, Rrstd, Rcol], writes=[Rdst])

            def post_norm_add(gi, m3, Rm, NT, sq, Rsq):
                rms_stats(lambda c: m3[:, c, 0:NT], Rm, NT, sq, Rsq)
                xv = x3(NT)
                for c in range(8):
                    k = tmp_rr[0] % 2
                    tmp_rr[0] += 1
                    S.op("dve", lambda e, c=c, k=k: e.scalar_tensor_tensor(tmpA[k][:, 0:NT], m3[:, c, 0:NT], col(gi, c),
                                                                           rstd[:, 0:NT], ALU.mult, ALU.mult),
                         reads=[Rm, Rrstd, Rcol], writes=[Rtmp[k]])
                    S.op("pool", lambda e, c=c, k=k: e.tensor_tensor(xv[:, c, :], xv[:, c, :], tmpA[k][:, 0:NT], ALU.add),
                         reads=[Rtmp[k], Rx], writes=[Rx])

            def proj_fm(slab_fn, src3, Rsrc, NT, n_out_chunks, evac, kc=8):
                for oc in range(n_out_chunks):
                    lhs_fn, Rw = slab_fn(oc)
                    if lhs_fn is None:
                        continue
                    pb, rp_ = bank()
                    S.group("pe", [MM(pb[:, 0:NT], lhs_fn(k), src3[:, k, 0:NT], k == 0, k == kc - 1) for k in range(kc)],
                            reads=[Rsrc, Rw], writes=[rp_])
                    evac(oc, pb, rp_)

            def mlp(li, NT):
                if 'mlp' not in DBG:
                    return
                phase()
                sq = AR.bf16(8 * 512).rearrange("p (c t) -> p c t", c=8)
                Rsq = ares("sq")
                hT = AR.bf16(8 * 512).rearrange("p (c t) -> p c t", c=8)
                Rh = ares("hT")
                aT = AR.bf16(32 * 512).rearrange("p (c t) -> p c t", c=32)
                Ra = ares("aT")
                mT = AR.f32(8 * 512).rearrange("p (c t) -> p c t", c=8)
                Rm = ares("mT")
                rl = [AR.f32(512), AR.f32(512)]
                Rrl = [ares("rl0"), ares("rl1")]
                rms_stats(lambda c: x3(NT)[:, c, :], Rx, NT, sq, Rsq)
                norm_apply(li * 4 + 2, lambda c: hT[:, c, 0:NT], Rh, NT)
                w1 = WB["mlp_w1"][li]
                w2 = WB["mlp_w2"][li]
                cur = {}

                def slab1(oc):
                    if oc % 4 == 0:
                        cur["v"], cur["r"] = wslab_k(w1, oc * 128, 512)
                    v = cur["v"]
                    return (lambda k, v=v, o=oc % 4: v[:, k, o * 128:(o + 1) * 128]) if v is not None else None, cur["r"]

                def evac1(oc, pb, rp_):
                    k = oc % 2
                    S.op("act", lambda e, pb=pb, k=k: e.activation(out=rl[k][:, 0:NT], in_=pb[:, 0:NT], func=AF.Relu),
                         reads=[rp_], writes=[Rrl[k]])
                    S.op("pool", lambda e, k=k, oc=oc: e.tensor_tensor(aT[:, oc, 0:NT], rl[k][:, 0:NT], rl[k][:, 0:NT], ALU.mult),
                         reads=[Rrl[k]], writes=[Ra])

                proj_fm(slab1, hT, Rh, NT, 32, evac1)

                def evac2(oc, pb, rp_):
                    S.op("act", lambda e, pb=pb, oc=oc: e.activation(out=mT[:, oc, 0:NT], in_=pb[:, 0:NT], func=AF.Copy),
                         reads=[rp_], writes=[Rm])

                w2v = w2.rearrange("(k p) o -> p k o", p=128)
                for half in range(2):
                    banks = [bank() for _ in range(4)]
                    for ks in range(4):
                        v, r = wget(w2v[:, ks * 8:(ks + 1) * 8, half * 512:(half + 1) * 512], (128, 8, 512))
                        if v is None:
                            continue
                        for o in range(4):
                            pb, rp_ = banks[o]
                            S.group("pe", [MM(pb[:, 0:NT], v[:, k, o * 128:(o + 1) * 128], aT[:, ks * 8 + k, 0:NT],
                                              ks == 0 and k == 0, ks == 3 and k == 7) for k in range(8)],
                                    reads=[Ra, r], writes=[rp_])
                    for o in range(4):
                        evac2(half * 4 + o, *banks[o])
                post_norm_add(li * 4 + 3, mT, Rm, NT, sq, Rsq)

            def attention(li, j, NT, st, first_mask, last_out):
                if 'attn' not in DBG:
                    return
                phase()
                QB = min(128, NT)
                NQB = NT // QB
                mflat = AR.f32(8 * 512)
                mT = mflat.rearrange("p (c t) -> p c t", c=8)
                Rm = ares("mT")
                hT = mflat[:, 0:2048].bitcast(BF16).rearrange("p (c t) -> p c t", c=8)
                sq = mflat[:, 2048:4096].bitcast(BF16).rearrange("p (c t) -> p c t", c=8)
                qT = AR.bf16(16 * 512)[0:64, :].rearrange("p (h t) -> p h t", h=16)
                Rq = ares("qT")
                kT = AR.bf16(4 * 640)[0:64, :].rearrange("p (h t) -> p h t", h=4)
                Rk = ares("kT")
                Vb = AR.bf16(5 * 256).rearrange("p (b v) -> p b v", b=5)
                Rv = ares("Vb")
                kvtok = kvst[:, :]
                Rkv = res("kvst")
                sc = [AR.f32(512), AR.f32(512)]
                Rsc = [ares("sc0"), ares("sc1")]
                PT = [AR.bf16(512), AR.bf16(512), AR.bf16(512), AR.bf16(512)]
                RPT = [ares(f"PT{i}") for i in range(4)]
                dn = AR.f32(512)
                Rdn = ares("dn")
                OT = AR.bf16(16 * 512)[0:64, :].rearrange("p (h t) -> p h t", h=16)
                Ro = ares("OT")
                Rkc, Rvc = res(f"kcar{st}{j}"), res(f"vcar{st}{j}")
                kc3 = kcar[st][j][:, :].rearrange("p (h t) -> p h t", h=4)
                S.op("dve", lambda e: e.tensor_copy(kT[:, :, 0:128], kc3), reads=[Rkc], writes=[Rk])
                S.op("dve", lambda e: e.tensor_copy(Vb[:, 0, :], vcar[st][j][:, :]), reads=[Rvc], writes=[Rv])
                rms_stats(lambda c: x3(NT)[:, c, :], Rx, NT, sq, Rm)
                norm_apply(li * 4 + 0, lambda c: hT[:, c, 0:NT], Rm, NT)
                wq = WB["a_w_qkv"][j]
                sl = []
                for g3 in range(3):
                    sl.append(wslab_k(wq, g3 * 512, 512))
                for h in range(20):
                    v, rw = sl[h // 8]
                    pb, rp_ = bank()
                    o = (h % 8) * 64
                    S.group("pe", [MM(pb[0:64, 0:NT], v[:, k, o:o + 64], hT[:, k, 0:NT], k == 0, k == 7) for k in range(8)]
                            if v is not None else [], reads=[Rm, rw], writes=[rp_])
                    if h < 16:
                        S.op("act", lambda e, pb=pb, h=h: e.mul(qT[:, h, 0:NT], pb[0:64, 0:NT], DH ** -0.5), reads=[rp_], writes=[Rq])
                    else:
                        S.op("act", lambda e, pb=pb, h=h: e.activation(out=kT[:, h - 16, 128:128 + NT], in_=pb[0:64, 0:NT],
                                                                        func=AF.Copy), reads=[rp_], writes=[Rk])
                v2, rw2 = sl[2]
                for qb in range(NQB):
                    pb, rp_ = bank()
                    S.group("pe", [MM(pb[0:QB, 0:512], hT[:, k, qb * QB:(qb + 1) * QB], v2[:, k, 0:512], k == 0, k == 7)
                                   for k in range(8)] if v2 is not None else [], reads=[Rm, rw2], writes=[rp_])
                    S.op("dve", lambda e, pb=pb, qb=qb: e.tensor_copy(Vb[0:QB, qb + 1, :], pb[0:QB, 256:512]),
                         reads=[rp_], writes=[Rv])
                    if last_out is not None and qb == NQB - 1 and 'nokvout' not in DBG:
                        S.op("act", lambda e, pb=pb: e.activation(out=kvtok[0:QB, :], in_=pb[0:QB, 0:512], func=AF.Copy),
                             reads=[rp_], writes=[Rkv])
                        ko, vo, cko, cvo = last_out
                        if 'nokvdma' in DBG:
                            pass
                        elif cko is None:
                            S.dma("sp", "yout1", [(ko, kvtok[:, 0:256]), (vo, kvtok[:, 256:512])], reads=[Rkv])
                        else:
                            S.dma("sp", "yout1", [(ko[WIN - QB:WIN, :], kvtok[0:QB, 0:256]), (vo[WIN - QB:WIN, :], kvtok[0:QB, 256:512]),
                                                 (ko[0:WIN - QB, :], cko[QB:WIN, :]), (vo[0:WIN - QB, :], cvo[QB:WIN, :])],
                                  reads=[Rkv])
                es4 = es_t[0:64, j * 16:(j + 1) * 16]
                pt_state = [0]

                def stage_a(qb, kvh):
                    pt_rr = pt_state[0]
                    kbs = []
                    for kb in range(2):
                        if kb == 0 and qb == 0 and first_mask == "skip":
                            continue
                        kbs.append(kb)
                    pts = []
                    for kb in kbs:
                        KB = 128 if kb == 0 else QB
                        c0 = qb * 128 if kb == 0 else 128 + qb * QB
                        pb, rp_ = bank()
                        S.group("pe", [MM(pb[0:KB, 0:4 * QB], kT[:, kvh, c0:c0 + KB], qT[:, 4 * kvh:4 * kvh + 4, qb * QB:(qb + 1) * QB],
                                          True, True)], reads=[Rk, Rq], writes=[rp_])
                        si = pt_rr % 2
                        pi = pt_rr % 4
                        pt_rr += 1
                        b3 = biasT[kb][:, :].rearrange("p (h q) -> p h q", h=16)[0:KB, 4 * kvh:4 * kvh + 4, 0:QB]
                        S.op("dve", lambda e, pb=pb, si=si, KB=KB, b3=b3: e.tensor_tensor(
                            sc[si][0:KB, 0:4 * QB].rearrange("p (g q) -> p g q", g=4),
                            pb[0:KB, 0:4 * QB].rearrange("p (g q) -> p g q", g=4), b3, ALU.add),
                             reads=[rp_, Rb[kb]], writes=[Rsc[si]])
                        if kb == 0 and qb == 0 and first_mask == "data":
                            bias_ap, rbias = pmk[0:KB, 0:1], Rpm
                        else:
                            bias_ap, rbias = cst[0:KB, K_ZERO:K_ZERO + 1], Rc
                        S.op("act", lambda e, si=si, pi=pi, KB=KB, bias_ap=bias_ap: e.activation(
                            out=PT[pi][0:KB, 0:4 * QB], in_=sc[si][0:KB, 0:4 * QB], func=AF.Exp, bias=bias_ap),
                             reads=[Rsc[si], rbias], writes=[RPT[pi]])
                        pts.append((kb, KB, pi))
                    pt_state[0] = pt_rr
                    return pts

                def stage_b(qb, kvh, pts):
                    pbo, rpo = bank()
                    S.group("pe", [MM(pbo[0:64, 0:4 * QB], Vb[0:KB, qb + kb, kvh * 64:(kvh + 1) * 64], PT[pi][0:KB, 0:4 * QB],
                                      n == 0, n == len(pts) - 1) for n, (kb, KB, pi) in enumerate(pts)],
                            reads=[Rv] + [RPT[pi] for (_, _, pi) in pts], writes=[rpo])
                    pbd, rpd = bank()
                    S.group("pe", [MM(pbd[0:64, 0:4 * QB], ones_bf[0:KB, 0:64], PT[pi][0:KB, 0:4 * QB],
                                      n == 0, n == len(pts) - 1) for n, (kb, KB, pi) in enumerate(pts)],
                            reads=[res("ones_bf")] + [RPT[pi] for (_, _, pi) in pts], writes=[rpd])
                    S.op("dve", lambda e, pbd=pbd, kvh=kvh: e.tensor_tensor(
                        dn[0:64, 0:4 * QB].rearrange("p (g q) -> p g q", g=4),
                        pbd[0:64, 0:4 * QB].rearrange("p (g q) -> p g q", g=4),
                        es4[:, 4 * kvh:4 * kvh + 4].unsqueeze(2).to_broadcast([64, 4, QB]), ALU.add),
                         reads=[rpd, Res_es], writes=[Rdn])
                    S.op("dve", lambda e: e.reciprocal(dn[0:64, 0:4 * QB], dn[0:64, 0:4 * QB]), reads=[Rdn], writes=[Rdn])
                    S.op("dve", lambda e, pbo=pbo, kvh=kvh, qb=qb: e.tensor_tensor(
                        OT[:, 4 * kvh:4 * kvh + 4, qb * QB:(qb + 1) * QB],
                        pbo[0:64, 0:4 * QB].rearrange("p (g q) -> p g q", g=4),
                        dn[0:64, 0:4 * QB].rearrange("p (g q) -> p g q", g=4), ALU.mult),
                         reads=[rpo, Rdn], writes=[Ro])

                prev_it = None
                for qb in range(NQB):
                    for kvh in range(NKV):
                        pts_ = stage_a(qb, kvh)
                        if prev_it is not None:
                            stage_b(*prev_it)
                        prev_it = (qb, kvh, pts_)
                if prev_it is not None:
                    stage_b(*prev_it)
                if ATT < 4:
                    return
                if NT >= 128:
                    S.op("dve", lambda e: e.tensor_copy(kc3, kT[:, :, NT:NT + 128]), reads=[Rk], writes=[Rkc])
                    S.op("dve", lambda e: e.tensor_copy(vcar[st][j][:, :], Vb[:, NQB, :]), reads=[Rv], writes=[Rvc])
                wo = WB["a_w_o"][j].rearrange("(h p) o -> p h o", p=64)
                cur = {}

                def slabo(oc):
                    if oc % 2 == 0:
                        cur["v"], cur["r"] = wget(wo[:, :, oc * 128:oc * 128 + 256], (64, 16, 256))
                    v = cur["v"]
                    return (lambda k, v=v, o=oc % 2: v[:, k, o * 128:(o + 1) * 128]) if v is not None else None, cur["r"]

                def evaco(oc, pb, rp_):
                    S.op("act", lambda e, pb=pb, oc=oc: e.activation(out=mT[:, oc, 0:NT], in_=pb[:, 0:NT], func=AF.Copy),
                         reads=[rp_], writes=[Rm])

                proj_fm(slabo, OT, Ro, NT, 8, evaco, kc=16)
                sq2 = AR.bf16(8 * 512).rearrange("p (c t) -> p c t", c=8)
                post_norm_add(li * 4 + 1, mT, Rm, NT, sq2, ares("sq2"))

            def conv(li, NT, st, out_ap):
                if 'conv' not in DBG:
                    return
                phase()
                sq = AR.bf16(8 * 512).rearrange("p (c t) -> p c t", c=8)
                Rsq = ares("sq")
                hT = AR.bf16(8 * 512).rearrange("p (c t) -> p c t", c=8)
                Rh = ares("hT")
                mT = AR.f32(8 * 512).rearrange("p (c t) -> p c t", c=8)
                Rm = ares("mT")
                u = AR.f32(8 * 514).rearrange("p (c t) -> p c t", c=8)
                Ru = ares("u")
                zT = AR.bf16(8 * 512).rearrange("p (c t) -> p c t", c=8)
                Rz = ares("zT")
                cg = [AR.f32(512), AR.f32(512)]
                Rcg = [ares("cg0"), ares("cg1")]
                yv = [AR.f32(512), AR.f32(512)]
                Ry = [ares("yv0"), ares("yv1")]
                Ruc = res(f"ucar{st}")
                uc3 = ucar[st][:, :].rearrange("p (c t) -> p c t", c=8)
                S.op("pool", lambda e: e.tensor_copy(u[:, :, 0:2], uc3), reads=[Ruc], writes=[Ru])
                rms_stats(lambda c: x3(NT)[:, c, :], Rx, NT, sq, Rsq)
                norm_apply(li * 4 + 0, lambda c: hT[:, c, 0:NT], Rh, NT)
                win = WB["c_w_in"]
                sl = {}
                for c in range(8):
                    if c % 4 == 0:
                        for part in range(3):
                            sl[part] = wslab_k(win, part * 1024 + c * 128, 512)
                    pbs = []
                    for part in range(3):
                        v, rw = sl[part]
                        pb, rp_ = bank()
                        o = (c % 4) * 128
                        S.group("pe", [MM(pb[:, 0:NT], v[:, k, o:o + 128], hT[:, k, 0:NT], k == 0, k == 7) for k in range(8)]
                                if v is not None else [], reads=[Rh, rw], writes=[rp_])
                        pbs.append((pb, rp_))
                    k2 = c % 2
                    (pbg, rbg), (pcg, rcg), (phh, rhh) = pbs
                    S.op("act", lambda e, pcg=pcg, k2=k2: e.activation(out=cg[k2][:, 0:NT], in_=pcg[:, 0:NT], func=AF.Copy),
                         reads=[rcg], writes=[Rcg[k2]])
                    S.op("dve", lambda e, phh=phh, k2=k2, c=c: e.tensor_tensor(u[:, c, 2:2 + NT], cg[k2][:, 0:NT], phh[:, 0:NT], ALU.mult),
                         reads=[Rcg[k2], rhh], writes=[Ru])
                    S.op("pool", lambda e, k2=k2, c=c: e.tensor_scalar(yv[k2][:, 0:NT], u[:, c, 0:NT], col(V_CW, c), None, ALU.mult),
                         reads=[Ru, Rcol], writes=[Ry[k2]])
                    S.op("dve", lambda e, k2=k2, c=c: e.scalar_tensor_tensor(yv[k2][:, 0:NT], u[:, c, 1:1 + NT], col(V_CW + 1, c),
                                                                              yv[k2][:, 0:NT], ALU.mult, ALU.add),
                         reads=[Ru, Rcol, Ry[k2]], writes=[Ry[k2]])
                    S.op("dve", lambda e, k2=k2, c=c: e.scalar_tensor_tensor(yv[k2][:, 0:NT], u[:, c, 2:2 + NT], col(V_CW + 2, c),
                                                                              yv[k2][:, 0:NT], ALU.mult, ALU.add),
                         reads=[Ru, Rcol, Ry[k2]], writes=[Ry[k2]])
                    S.op("dve", lambda e, pbg=pbg, k2=k2, c=c: e.tensor_tensor(zT[:, c, 0:NT], yv[k2][:, 0:NT], pbg[:, 0:NT], ALU.mult),
                         reads=[Ry[k2], rbg], writes=[Rz])
                S.op("pool", lambda e: e.tensor_copy(uc3, u[:, :, NT:NT + 2]), reads=[Ru], writes=[Ruc])
                if out_ap is not None:
                    S.dma("sp", "yout1", [(out_ap[:, :], ucar[st][:, :])], reads=[Ruc])
                wout = WB["c_w_out"]
                cur = {}

                def slabo(oc):
                    if oc % 4 == 0:
                        cur["v"], cur["r"] = wslab_k(wout, oc * 128, 512)
                    v = cur["v"]
                    return (lambda k, v=v, o=oc % 4: v[:, k, o * 128:(o + 1) * 128]) if v is not None else None, cur["r"]

                def evaco(oc, pb, rp_):
                    S.op("act", lambda e, pb=pb, oc=oc: e.activation(out=mT[:, oc, 0:NT], in_=pb[:, 0:NT], func=AF.Copy),
                         reads=[rp_], writes=[Rm])

                proj_fm(slabo, zT, Rz, NT, 8, evaco)
                post_norm_add(li * 4 + 1, mT, Rm, NT, sq, Rsq)

            def rwkv(li, NT, st, state_only, outs):
                if 'rwkv' not in DBG:
                    return
                phase()
                C = min(64, NT)
                NTC = NT // C
                regA = AR.f32(8 * 513 + 8 * 512)
                h32 = regA[:, 0:8 * 513].rearrange("p (c t) -> p c t", c=8)
                Rh32 = ares("h32")
                xxf = regA[:, 8 * 513:8 * 513 + 8 * 512]
                xx = xxf.rearrange("p (c t) -> p c t", c=8)
                Rxx = ares("xx")
                xm = [AR.bf16(8 * 512).rearrange("p (c t) -> p c t", c=8) for _ in range(3)]
                Rxm = [ares(f"xm{i}") for i in range(3)]
                tw = AR.bf16(512)
                ta = AR.bf16(512)
                tg = AR.bf16(512)
                Rlo = ares("lora")
                zT = AR.bf16(8 * 512).rearrange("p (c t) -> p c t", c=8)
                Rz = ares("zT")
                Rsh = res(f"shc{st}")
                RH = res(f"H{st}")
                H3 = Hst[st][:, :].rearrange("p (c j) -> p c j", c=8)
                sq = xxf[:, 0:2048].bitcast(BF16).rearrange("p (c t) -> p c t", c=8)
                rms_stats(lambda c: x3(NT)[:, c, :], Rx, NT, sq, Rxx)
                S.op("pool", lambda e: e.tensor_copy(h32[:, :, 0:1], shc[st][:, :].unsqueeze(2)), reads=[Rsh], writes=[Rh32])
                norm_apply(li * 4 + 0, lambda c: h32[:, c, 1:1 + NT], Rh32, NT)
                S.op("pool", lambda e: e.tensor_copy(shc[st][:, :].unsqueeze(2), h32[:, :, NT:NT + 1]), reads=[Rh32], writes=[Rsh])
                if outs is not None:
                    S.dma("sp", "yout1", [(outs[1][:, :], shc[st][:, :])], reads=[Rsh])
                for c in range(8):
                    S.op("pool", lambda e, c=c: e.tensor_tensor(xx[:, c, 0:NT], h32[:, c, 0:NT], h32[:, c, 1:1 + NT], ALU.subtract),
                         reads=[Rh32], writes=[Rxx])

                def mix(mi, dst, Rdst):
                    for c in range(8):
                        S.op("dve", lambda e, c=c: e.scalar_tensor_tensor(
                            dst[:, c, 0:NT], xx[:, c, 0:NT], col(V_MU + mi, c), h32[:, c, 1:1 + NT], ALU.mult, ALU.add),
                             reads=[Rxx, Rh32, Rcol], writes=[Rdst])

                lw1v = lw1[:, :].rearrange("p (k o) -> p k o", k=8)
                mix(1, xm[0], Rxm[0])
                mix(4, xm[1], Rxm[1])
                mix(5, xm[2], Rxm[2])
                for (src, Rs, o0, M, dst, fn) in ((xm[0], Rxm[0], 0, 64, tw, AF.Tanh), (xm[1], Rxm[1], 64, 64, ta, AF.Copy),
                                                  (xm[2], Rxm[2], 128, 128, tg, AF.Sigmoid)):
                    pb, rp_ = bank()
                    S.group("pe", [MM(pb[0:M, 0:NT], lw1v[:, k, o0:o0 + M], src[:, k, 0:NT], k == 0, k == 7) for k in range(8)],
                            reads=[Rs, Rlw], writes=[rp_])
                    S.op("act", lambda e, pb=pb, M=M, dst=dst, fn=fn: e.activation(out=dst[0:M, 0:NT], in_=pb[0:M, 0:NT], func=fn),
                         reads=[rp_], writes=[Rlo])
                mix(0, xm[0], Rxm[0])
                mix(2, xm[1], Rxm[1])
                mix(3, xm[2], Rxm[2])
                S.barrier()
                sub = [0]

                def T2(n=512):
                    v = regA[:, sub[0]:sub[0] + n]
                    sub[0] += n
                    assert sub[0] <= 8 * 513 + 8 * 512
                    return v
                rT, kTt, aTt, sg, csum, kk, t2, bon, vv = [T2() for _ in range(9)]
                t3 = aTt
                arB = T2(1024).rearrange("p (a t) -> p a t", a=2)
                bb, kbar, btl, ktl = [T2() for _ in range(4)]
                sqb = T2(256).bitcast(BF16)
                gam = T2(8)
                Lm = AR.f32(512).rearrange("p (n s) -> p n s", n=8)
                MA = [AR.f32(1024).rearrange("p (n s) -> p n s", n=8) for _ in range(2)]
                Xp = [AR.f32(512).rearrange("p (n s) -> p n s", n=8) for _ in range(2)]
                Mp = [AR.f32(512).rearrange("p (n s) -> p n s", n=8) for _ in range(2)]
                Pp = [AR.f32(512).rearrange("p (n s) -> p n s", n=8) for _ in range(2)]
                Btk = AR.f32(512).rearrange("p (n s) -> p n s", n=8)
                Ktk = AR.f32(512).rearrange("p (n s) -> p n s", n=8)
                Vtk = AR.f32(512).rearrange("p (n s) -> p n s", n=8)
                Wb = AR.f32(64)
                Ub = AR.f32(64)
                ysb = AR.f32(512)
                RT = {n: ares("rw_" + n) for n in ("r", "k", "a", "sg", "cs", "kk", "t2", "t3", "bon", "vv", "ar", "bb", "kb", "bt",
                                                  "kt", "sqb", "gam", "L", "MA0", "MA1", "X0", "X1", "M0", "M1", "P0", "P1", "Bt",
                                                  "Kt", "Vt", "Wb", "Ub", "ysb")}
                RT["t3"] = RT["a"]
                wr = WB["b_w_rkv"]
                sl = {}
                Rbd = res("bd_bf")
                for c in range(8):
                    if c % 4 == 0:
                        for part in range(3):
                            sl[part] = wslab_k(wr[part], c * 128, 512)
                    o = (c % 4) * 128
                    pr = []
                    for part in range(3):
                        if part == 0 and state_only:
                            pr.append((None, None))
                            continue
                        v, rw = sl[part]
                        pb, rp_ = bank()
                        S.group("pe", [MM(pb[:, 0:NT], v[:, k, o:o + 128], xm[part][:, k, 0:NT], k == 0, k == 7) for k in range(8)]
                                if v is not None else [], reads=[Rxm[part], rw], writes=[rp_])
                        pr.append((pb, rp_))
                    (pr_r, rr_r), (pr_k, rr_k), (pr_v, rr_v) = pr
                    if not state_only:
                        S.op("act", lambda e, p=pr_r: e.activation(out=rT[:, 0:NT], in_=p[:, 0:NT], func=AF.Copy), reads=[rr_r], writes=[RT["r"]])
                    S.op("act", lambda e, p=pr_k: e.activation(out=kTt[:, 0:NT], in_=p[:, 0:NT], func=AF.Copy), reads=[rr_k], writes=[RT["k"]])
                    S.op("act", lambda e, p=pr_v: e.activation(out=vv[:, 0:NT], in_=p[:, 0:NT], func=AF.Copy), reads=[rr_v], writes=[RT["vv"]])
                    pb, rp_ = bank()
                    S.group("pe", [MM(pb[:, 0:NT], lw2[:, c * 128:(c + 1) * 128], tw[0:64, 0:NT], True, True)], reads=[Rlo, Rlw], writes=[rp_])
                    S.op("act", lambda e, pb=pb, c=c: e.activation(out=sg[:, 0:NT], in_=pb[:, 0:NT], func=AF.Sigmoid, bias=col(V_W0, c)),
                         reads=[rp_, Rcol], writes=[RT["sg"]])
                    pb, rp_ = bank()
                    S.group("pe", [MM(pb[:, 0:NT], lw2[:, 1024 + c * 128:1024 + (c + 1) * 128], ta[0:64, 0:NT], True, True)], reads=[Rlo, Rlw], writes=[rp_])
                    S.op("act", lambda e, pb=pb, c=c: e.activation(out=aTt[:, 0:NT], in_=pb[:, 0:NT], func=AF.Sigmoid, bias=col(V_A0, c)),
                         reads=[rp_, Rcol], writes=[RT["a"]])
                    S.op("dve", lambda e, c=c: e.tensor_scalar(kk[:, 0:NT], kTt[:, 0:NT], col(V_KK, c), None, ALU.mult),
                         reads=[RT["k"], Rcol], writes=[RT["kk"]])
                    S.op("act", lambda e: e.activation(out=sqb[:, 0:NT], in_=kk[:, 0:NT], func=AF.Square), reads=[RT["kk"]], writes=[RT["sqb"]])
                    pb, rp_ = bank()
                    S.group("pe", [MM(pb[:, 0:NT], bd_bf[:, :], sqb[:, 0:NT], True, True)], reads=[RT["sqb"], Rbd], writes=[rp_])
                    S.op("act", lambda e, pb=pb: e.activation(out=t2[:, 0:NT], in_=pb[:, 0:NT], func=AF.Sqrt, bias=cs(K_ZERO + 1)),
                         reads=[rp_, Rc], writes=[RT["t2"]])
                    S.op("dve", lambda e: e.reciprocal(t2[:, 0:NT], t2[:, 0:NT]), reads=[RT["t2"]], writes=[RT["t2"]])
                    S.op("dve", lambda e: e.tensor_tensor(kk[:, 0:NT], kk[:, 0:NT], t2[:, 0:NT], ALU.mult), reads=[RT["kk"], RT["t2"]], writes=[RT["kk"]])
                    S.op("pool", lambda e, c=c: e.tensor_scalar(t2[:, 0:NT], aTt[:, 0:NT], col(V_KA, c), omk[:, c:c + 1], ALU.mult, ALU.add),
                         reads=[RT["a"], Rcol, res("omk"), RT["t2"]], writes=[RT["t2"]])
                    S.op("pool", lambda e: e.tensor_tensor(kTt[:, 0:NT], kTt[:, 0:NT], t2[:, 0:NT], ALU.mult), reads=[RT["k"], RT["t2"]], writes=[RT["k"]])
                    S.op("pool", lambda e: e.tensor_tensor(t3[:, 0:NT], kk[:, 0:NT], aTt[:, 0:NT], ALU.mult), reads=[RT["kk"], RT["a"]], writes=[RT["t3"]])
                    if not state_only:
                        S.op("dve", lambda e, c=c: e.scalar_tensor_tensor(sqb[:, 0:NT], rT[:, 0:NT], col(V_RK, c), kTt[:, 0:NT], ALU.mult, ALU.mult),
                             reads=[RT["r"], RT["k"], Rcol, RT["sqb"]], writes=[RT["sqb"]])
                        pb, rp_ = bank()
                        S.group("pe", [MM(pb[:, 0:NT], bd_bf[:, :], sqb[:, 0:NT], True, True)], reads=[RT["sqb"], Rbd], writes=[rp_])
                        S.op("dve", lambda e, pb=pb: e.tensor_tensor(bon[:, 0:NT], pb[:, 0:NT], vv[:, 0:NT], ALU.mult),
                             reads=[rp_, RT["vv"]], writes=[RT["bon"]])
                    S.op("dve", lambda e: e.tensor_tensor_scan(csum[:, 0:NT], cst[:, K_SEG:K_SEG + NT], sg[:, 0:NT], 0.0, ALU.mult, ALU.add),
                         reads=[RT["sg"], Rc], writes=[RT["cs"]])
                    cs3 = csum[:, 0:NT].rearrange("p (n t) -> p n t", n=NTC)
                    S.op("act", lambda e: e.activation(out=t2[:, 0:NT], in_=csum[:, 0:NT], func=AF.Exp, scale=-C0), reads=[RT["cs"], RT["t2"]], writes=[RT["t2"]])
                    S.op("dve", lambda e: e.tensor_copy(gam[:, 0:NTC].unsqueeze(2), t2[:, 0:NT].rearrange("p (n t) -> p n t", n=NTC)[:, :, C - 1:C]),
                         reads=[RT["t2"]], writes=[RT["gam"]])
                    if not state_only:
                        S.op("dve", lambda e: e.tensor_tensor(arB[:, 1, 0:NT], rT[:, 0:NT], t2[:, 0:NT], ALU.mult), reads=[RT["r"], RT["t2"]], writes=[RT["ar"]])
                    S.op("pool", lambda e: e.tensor_tensor(t2[:, 0:NT], csum[:, 0:NT], sg[:, 0:NT], ALU.subtract), reads=[RT["cs"], RT["sg"], RT["t2"]], writes=[RT["t2"]])
                    S.op("act", lambda e: e.activation(out=t2[:, 0:NT], in_=t2[:, 0:NT], func=AF.Exp, scale=-C0), reads=[RT["t2"]], writes=[RT["t2"]])
                    S.op("dve", lambda e: e.scalar_tensor_tensor(arB[:, 0, 0:NT], kk[:, 0:NT], -1.0, t2[:, 0:NT], ALU.mult, ALU.mult),
                         reads=[RT["kk"], RT["t2"]], writes=[RT["ar"]])
                    S.op("act", lambda e: e.activation(out=t2[:, 0:NT], in_=csum[:, 0:NT], func=AF.Exp, scale=C0), reads=[RT["cs"], RT["t2"]], writes=[RT["t2"]])
                    S.op("dve", lambda e: e.tensor_tensor(bb[:, 0:NT], t3[:, 0:NT], t2[:, 0:NT], ALU.mult), reads=[RT["t3"], RT["t2"]], writes=[RT["bb"]])
                    S.op("pool", lambda e: e.tensor_tensor(kbar[:, 0:NT], kTt[:, 0:NT], t2[:, 0:NT], ALU.mult), reads=[RT["k"], RT["t2"]], writes=[RT["kb"]])
                    S.op("dve", lambda e: e.tensor_tensor(t2[:, 0:NT].rearrange("p (n t) -> p n t", n=NTC),
                                                          cs3[:, :, C - 1:C].to_broadcast([128, NTC, C]), cs3, ALU.subtract),
                         reads=[RT["cs"], RT["t2"]], writes=[RT["t2"]])
                    S.op("act", lambda e: e.activation(out=t2[:, 0:NT], in_=t2[:, 0:NT], func=AF.Exp, scale=-C0), reads=[RT["t2"]], writes=[RT["t2"]])
                    S.op("dve", lambda e: e.tensor_tensor(btl[:, 0:NT], t3[:, 0:NT], t2[:, 0:NT], ALU.mult), reads=[RT["t3"], RT["t2"]], writes=[RT["bt"]])
                    S.op("pool", lambda e: e.tensor_tensor(ktl[:, 0:NT], kTt[:, 0:NT], t2[:, 0:NT], ALU.mult), reads=[RT["k"], RT["t2"]], writes=[RT["kt"]])

                    def hs(hh):
                        return slice(64 * hh, 64 * hh + 64)

                    def tcs(n):
                        return slice(n * C, (n + 1) * C)

                    def ps_(hh):
                        return slice(64 * hh, 64 * hh + C)
                    pb, rp_ = bank()
                    S.group("pe", [MM(pb[ps_(hh), n * 64:n * 64 + C], arB[hs(hh), 0, tcs(n)], bb[hs(hh), tcs(n)], True, True)
                                   for n in range(NTC) for hh in range(2)], reads=[RT["ar"], RT["bb"]], writes=[rp_])
                    S.op("dve", lambda e, pb=pb: e.tensor_tensor(Lm[:, 0:NTC, 0:C], pb[:, 0:NTC * 64].rearrange("p (n s) -> p n s", n=NTC)[:, :, 0:C],
                                                                 cst[:, K_ML:K_ML + C].unsqueeze(1).to_broadcast([128, NTC, C]), ALU.mult),
                         reads=[rp_, Rc], writes=[RT["L"]])
                    nA = 1 if state_only else 2
                    for mi, (lhs, Rl) in enumerate(((bb, RT["bb"]), (kbar, RT["kb"]))):
                        for half in range(2):
                            n0 = half * (NTC // 2 if NTC > 1 else 1)
                            n1 = NTC if (half == 1 or NTC == 1) else NTC // 2
                            if n0 >= n1:
                                continue
                            pb, rp_ = bank()
                            S.group("pe", [MM(pb[ps_(hh), (n - n0) * 128:(n - n0 + 1) * 128].rearrange("p (a t) -> p a t", a=2)[:, 0:nA, 0:C],
                                              lhs[hs(hh), tcs(n)], arB[hs(hh), 0:nA, tcs(n)], True, True)
                                           for n in range(n0, n1) for hh in range(2)], reads=[Rl, RT["ar"]], writes=[rp_])
                            for a_ in range(nA):
                                S.op("dve", lambda e, pb=pb, mi=mi, n0=n0, n1=n1, a_=a_: e.tensor_tensor(
                                    MA[mi][:, n0:n1, a_ * 64:a_ * 64 + C],
                                    pb[:, 0:(n1 - n0) * 128].rearrange("p (n x) -> p n x", x=128)[:, :, a_ * 64:a_ * 64 + C],
                                    cst[:, K_M1 + a_ * 64:K_M1 + a_ * 64 + C].unsqueeze(1).to_broadcast([128, n1 - n0, C]),
                                    ALU.mult), reads=[rp_, Rc], writes=[RT[f"MA{mi}"]])
                    for (src, Rs_, dst, Rd_) in ((btl, RT["bt"], Btk, RT["Bt"]), (ktl, RT["kt"], Ktk, RT["Kt"]), (vv, RT["vv"], Vtk, RT["Vt"])):
                        pb, rp_ = bank()
                        S.group("pe", [MM(pb[64 * hh:64 * hh + C, n * 64:(n + 1) * 64], src[hs(hh), tcs(n)], ident[hs(hh), hs(hh)], True, True)
                                       for n in range(NTC) for hh in range(2)], reads=[Rs_, Rc], writes=[rp_])
                        if C == 64:
                            S.op("act", lambda e, pb=pb, dst=dst: e.activation(out=dst[:, 0:NTC, :], in_=pb[:, 0:NTC * 64].rearrange("p (n s) -> p n s", n=NTC),
                                                                              func=AF.Copy), reads=[rp_], writes=[Rd_])
                        else:
                            for hh in range(2):
                                S.op("act", lambda e, pb=pb, dst=dst, hh=hh: e.activation(out=dst[64 * hh:64 * hh + C, 0, :], in_=pb[64 * hh:64 * hh + C, 0:64],
                                                                                         func=AF.Copy), reads=[rp_], writes=[Rd_])
                    S.op("pool", lambda e: e.tensor_tensor(Pp[0][:, 0:NTC, 0:C], MA[0][:, 0:NTC, 0:C],
                                                          cst[:, K_I2:K_I2 + C].unsqueeze(1).to_broadcast([128, NTC, C]), ALU.add),
                         reads=[RT["MA0"], Rc], writes=[RT["P0"]])
                    Xc, RXc = Lm, RT["L"]
                    Mc, RMc = MA[0], RT["MA0"]
                    pcur = 0
                    nlev = 5 if C == 64 else 4
                    for m in range(nlev):
                        Xn, RXn = Xp[m % 2], RT[f"X{m % 2}"]
                        Mn, RMn = Mp[m % 2], RT[f"M{m % 2}"]
                        last = (m == nlev - 1)
                        pbx, rpx = bank()
                        S.group("pe", [MM(pbx[ps_(hh), n * 64:n * 64 + C], Mc[ps_(hh), n, 0:C], Xc[ps_(hh), n, 0:C], True, True)
                                       for n in range(NTC) for hh in range(2)], reads=[RXc, RMc], writes=[rpx])
                        S.op("act", lambda e, pbx=pbx, Xn=Xn: e.activation(out=Xn[:, 0:NTC, 0:C], in_=pbx[:, 0:NTC * 64].rearrange("p (n s) -> p n s", n=NTC)[:, :, 0:C],
                                                                          func=AF.Copy), reads=[rpx], writes=[RXn])
                        if not last:
                            pbm, rpm = bank()
                            S.group("pe", [MM(pbm[ps_(hh), n * 64:n * 64 + C], Xc[ps_(hh), n, 0:C], Mc[ps_(hh), n, 0:C], True, True)
                                           for n in range(NTC) for hh in range(2)], reads=[RXc, RMc], writes=[rpm])
                            S.op("dve", lambda e, pbm=pbm, Mn=Mn: e.tensor_copy(Mn[:, 0:NTC, 0:C], pbm[:, 0:NTC * 64].rearrange("p (n s) -> p n s", n=NTC)[:, :, 0:C]),
                                 reads=[rpm], writes=[RMn])
                        pbp, rpp = bank()
                        S.group("pe", [MM(pbp[ps_(hh), n * 64:n * 64 + C], Xn[ps_(hh), n, 0:C], Pp[pcur][ps_(hh), n, 0:C], True, True)
                                       for n in range(NTC) for hh in range(2)], reads=[RXn, RT[f"P{pcur}"]], writes=[rpp])
                        S.op("dve", lambda e, pbp=pbp, pcur=pcur: e.tensor_tensor(
                            Pp[1 - pcur][:, 0:NTC, 0:C], pbp[:, 0:NTC * 64].rearrange("p (n s) -> p n s", n=NTC)[:, :, 0:C],
                            Pp[pcur][:, 0:NTC, 0:C], ALU.add), reads=[rpp, RT[f"P{pcur}"]], writes=[RT[f"P{1 - pcur}"]])
                        pcur = 1 - pcur
                        Xc, RXc = Xn, RXn
                        Mc, RMc = Mn, RMn
                    PT_, RPT_ = Pp[pcur], RT[f"P{pcur}"]
                    if not state_only:
                        pby, rpy = psb[7], RPS[7]
                    for n in range(NTC):
                        pw, rpw = bank()
                        fns = []
                        for hh in range(2):
                            fns.append(MM(pw[ps_(hh), 0:64], arB[hs(hh), 0, tcs(n)], H3[hs(hh), c, :], True, False))
                            fns.append(MM(pw[ps_(hh), 0:64], MA[1][ps_(hh), n, 0:C], Vtk[ps_(hh), n, :], False, True))
                        S.group("pe", fns, reads=[RT["ar"], RH, RT["MA1"], RT["Vt"]], writes=[rpw])
                        if C == 64:
                            S.op("act", lambda e, pw=pw: e.activation(out=Wb[:, 0:64], in_=pw[:, 0:64], func=AF.Copy), reads=[rpw], writes=[RT["Wb"]])
                        else:
                            for hh in range(2):
                                S.op("act", lambda e, pw=pw, hh=hh: e.activation(out=Wb[ps_(hh), 0:64], in_=pw[ps_(hh), 0:64], func=AF.Copy),
                                     reads=[rpw], writes=[RT["Wb"]])
                        pu, rpu = bank()
                        S.group("pe", [MM(pu[ps_(hh), 0:64], PT_[ps_(hh), n, 0:C], Wb[ps_(hh), 0:64], True, True) for hh in range(2)],
                                reads=[RPT_, RT["Wb"]], writes=[rpu])
                        if C == 64:
                            S.op("dve", lambda e, pu=pu: e.tensor_copy(Ub[:, 0:64], pu[:, 0:64]), reads=[rpu], writes=[RT["Ub"]])
                        else:
                            for hh in range(2):
                                S.op("dve", lambda e, pu=pu, hh=hh: e.tensor_copy(Ub[ps_(hh), 0:64], pu[ps_(hh), 0:64]), reads=[rpu], writes=[RT["Ub"]])
                        if not state_only:
                            fns = []
                            for hh in range(2):
                                oy = pby[hs(hh), n * C:(n + 1) * C]
                                fns.append(MM(oy, H3[hs(hh), c, :], arB[hs(hh), 1, tcs(n)], True, False))
                                fns.append(MM(oy, Ub[ps_(hh), 0:64], MA[0][ps_(hh), n, 64:64 + C], False, False))
                                fns.append(MM(oy, Vtk[ps_(hh), n, :], MA[1][ps_(hh), n, 64:64 + C], False, True))
                            S.group("pe", fns, reads=[RH, RT["ar"], RT["Ub"], RT["MA0"], RT["MA1"], RT["Vt"]], writes=[rpy])
                        ph, rph = bank()
                        fns = []
                        for hh in range(2):
                            fns.append(MM(ph[hs(hh), 0:64], Btk[ps_(hh), n, :], Ub[ps_(hh), 0:64], True, False))
                            fns.append(MM(ph[hs(hh), 0:64], Ktk[ps_(hh), n, :], Vtk[ps_(hh), n, :], False, True))
                        S.group("pe", fns, reads=[RT["Bt"], RT["Kt"], RT["Ub"], RT["Vt"]], writes=[rph])
                        S.op("dve", lambda e, ph=ph, n=n, c=c: e.scalar_tensor_tensor(H3[:, c, :], H3[:, c, :], gam[:, n:n + 1], ph[:, 0:64], ALU.mult, ALU.add),
                             reads=[rph, RT["gam"], RH], writes=[RH])
                    if not state_only:
                        S.op("act", lambda e, pby=pby: e.activation(out=ysb[:, 0:NT], in_=pby[:, 0:NT], func=AF.Copy), reads=[rpy], writes=[RT["ysb"]])
                        S.op("dve", lambda e: e.tensor_copy(sqb[:, 0:NT], ysb[:, 0:NT]), reads=[RT["ysb"], RT["sqb"]], writes=[RT["sqb"]])
                        pm_, rpm_ = bank()
                        S.group("pe", [MM(pm_[:, 0:NT], bd_bf[:, :], sqb[:, 0:NT], True, True)], reads=[RT["sqb"], Rbd], writes=[rpm_])
                        S.op("dve", lambda e, pm_=pm_: e.scalar_tensor_tensor(ysb[:, 0:NT], pm_[:, 0:NT], -1.0 / 64, ysb[:, 0:NT], ALU.mult, ALU.add),
                             reads=[rpm_, RT["ysb"]], writes=[RT["ysb"]])
                        S.op("act", lambda e: e.activation(out=sqb[:, 0:NT], in_=ysb[:, 0:NT], func=AF.Square), reads=[RT["ysb"], RT["sqb"]], writes=[RT["sqb"]])
                        pv_, rpv_ = bank()
                        S.group("pe", [MM(pv_[:, 0:NT], bd_bf[:, :], sqb[:, 0:NT], True, True)], reads=[RT["sqb"], Rbd], writes=[rpv_])
                        S.op("act", lambda e, pv_=pv_: e.activation(out=t2[:, 0:NT], in_=pv_[:, 0:NT], func=AF.Sqrt, bias=cs(K_GEPS), scale=1.0 / 64),
                             reads=[rpv_, Rc, RT["t2"]], writes=[RT["t2"]])
                        S.op("dve", lambda e: e.reciprocal(t2[:, 0:NT], t2[:, 0:NT]), reads=[RT["t2"]], writes=[RT["t2"]])
                        S.op("dve", lambda e: e.tensor_tensor(ysb[:, 0:NT], ysb[:, 0:NT], t2[:, 0:NT], ALU.mult), reads=[RT["ysb"], RT["t2"]], writes=[RT["ysb"]])
                        S.op("dve", lambda e, c=c: e.tensor_scalar(ysb[:, 0:NT], ysb[:, 0:NT], col(V_LNW, c), col(V_LNB, c), ALU.mult, ALU.add),
                             reads=[RT["ysb"], Rcol], writes=[RT["ysb"]])
                        S.op("pool", lambda e: e.tensor_tensor(ysb[:, 0:NT], ysb[:, 0:NT], bon[:, 0:NT], ALU.add), reads=[RT["ysb"], RT["bon"]], writes=[RT["ysb"]])
                        pg, rpg = bank()
                        S.group("pe", [MM(pg[:, 0:NT], lg2[:, c * 128:(c + 1) * 128], tg[:, 0:NT], True, True)], reads=[Rlo, Rlw], writes=[rpg])
                        S.op("dve", lambda e, pg=pg, c=c: e.tensor_tensor(zT[:, c, 0:NT], ysb[:, 0:NT], pg[:, 0:NT], ALU.mult),
                             reads=[rpg, RT["ysb"]], writes=[Rz])
                if outs is not None:
                    S.dma("sp", "yout1", [(outs[0][:, :], Hst[st][:, :])], reads=[RH])
                if state_only:
                    return
                S.barrier()
                mT3 = h32[:, :, 0:512]
                Rm = Rh32
                wo = WB["b_w_o"]
                cur = {}

                def slabo(oc):
                    if oc % 4 == 0:
                        cur["v"], cur["r"] = wslab_k(wo, oc * 128, 512)
                    v = cur["v"]
                    return (lambda k, v=v, o=oc % 4: v[:, k, o * 128:(o + 1) * 128]) if v is not None else None, cur["r"]

                def evaco(oc, pb, rp_):
                    S.op("act", lambda e, pb=pb, oc=oc: e.activation(out=mT3[:, oc, 0:NT], in_=pb[:, 0:NT], func=AF.Copy),
                         reads=[rp_], writes=[Rm])

                proj_fm(slabo, zT, Rz, NT, 8, evaco)
                post_norm_add(li * 4 + 1, mT3, Rm, NT, sq, Rxx)

            Rst = [res("stage0"), res("stage0")]
            st_rr = [0]

            def load_tile(src2d, t0, NT):
                QB = min(128, NT)
                xv = xT[:, :].rearrange("p (c t) -> p c t", c=8)
                for b in range(NT // QB):
                    k = st_rr[0] % 2
                    st_rr[0] += 1
                    S.dma("sp", "xin0", [(stage[k][0:QB, :], src2d[t0 + b * QB:t0 + (b + 1) * QB, :])], writes=[Rst[k]])
                    for half in range(2):
                        pb, rp_ = bank()
                        S.group("pe", [TR(pb[:, cc * QB:(cc + 1) * QB], stage[k][0:QB, (half * 4 + cc) * 128:(half * 4 + cc + 1) * 128], ident[0:QB, 0:QB])
                                       for cc in range(4)], reads=[Rst[k], Rc], writes=[rp_])
                        S.op("act" if half == 0 else "dve",
                             (lambda e, pb=pb, b=b, half=half: e.activation(out=xv[:, half * 4:half * 4 + 4, b * QB:(b + 1) * QB],
                                                                           in_=pb[:, 0:4 * QB].rearrange("p (c t) -> p c t", c=4), func=AF.Copy))
                             if half == 0 else
                             (lambda e, pb=pb, b=b, half=half: e.tensor_copy(xv[:, half * 4:half * 4 + 4, b * QB:(b + 1) * QB],
                                                                            pb[:, 0:4 * QB].rearrange("p (c t) -> p c t", c=4))),
                             reads=[rp_], writes=[Rx])

            def store_tile(dst2d, t0, NT):
                QB = min(128, NT)
                xv = xT[:, :].rearrange("p (c t) -> p c t", c=8)
                for b in range(NT // QB):
                    k = st_rr[0] % 2
                    st_rr[0] += 1
                    for half in range(2):
                        pb, rp_ = bank()
                        S.group("pe", [TR(pb[0:QB, cc * 128:(cc + 1) * 128], xv[:, half * 4 + cc, b * QB:(b + 1) * QB], ident)
                                       for cc in range(4)], reads=[Rx, Rc], writes=[rp_])
                        S.op("act" if half == 0 else "dve",
                             (lambda e, pb=pb, k=k, half=half: e.activation(out=stage[k][0:QB, half * 512:(half + 1) * 512], in_=pb[0:QB, 0:512], func=AF.Copy))
                             if half == 0 else
                             (lambda e, pb=pb, k=k, half=half: e.tensor_copy(stage[k][0:QB, half * 512:(half + 1) * 512], pb[0:QB, 0:512])),
                             reads=[rp_], writes=[Rst[k]])
                    S.dma("sp", "yout0", [(dst2d[t0 + b * QB:t0 + (b + 1) * QB, :], stage[k][0:QB, :])], reads=[Rst[k]])

            for l in range(2):
                S.op("pool", lambda e, l=l: e.memset(kcar[0][l][:, :], 0.0), writes=[res(f"kcar0{l}")])
                S.op("pool", lambda e, l=l: e.memset(vcar[0][l][:, :], 0.0), writes=[res(f"vcar0{l}")])
            S.op("pool", lambda e: e.memset(Hst[0][:, :], 0.0), writes=[res("H0")])
            S.op("pool", lambda e: e.memset(shc[0][:, :], 0.0), writes=[res("shc0")])
            S.op("pool", lambda e: e.memset(ucar[0][:, :], 0.0), writes=[res("ucar0")])
            S.dma("sp", "min", [(Hst[1][:, :], I["h0"][:, :]), (shc[1][:, :], I["sh0"][:, :]), (ucar[1][:, :], I["cv0"][:, :])],
                  writes=[res("H1"), res("shc1"), res("ucar1")])

            ntiles = NPRE + NMAIN
            for ti in range(ntiles):
                pre = ti < NPRE
                partial = pre and (ti < NPRE - 1) and not full_prefix
                last = ti == ntiles - 1
                load_tile(I["xp"], ti * 512, 512)
                fm = "skip" if ti == 0 else ("data" if ti == NPRE else None)
                if NL > 0:
                    attention(0, 0, 512, 0, fm, (O["akp"][0], O["avp"][0], None, None) if last else None)
                if NL > 0.5:
                    mlp(0, 512)
                if NL > 1:
                    rwkv(1, 512, 0, partial, (O["wkvp"], O["shp"]) if last else None)
                if partial:
                    continue
                if NL > 1.5:
                    mlp(1, 512)
                if ti == NPRE:
                    S.op("dve", lambda e: e.tensor_scalar(ucar[0][:, :], ucar[0][:, :], pflag[:, 0:1], None, ALU.mult),
                         reads=[res("ucar0"), res("pflag")], writes=[res("ucar0")])
                if NL > 2:
                    conv(2, 512, 0, O["cvp"] if last else None)
                if NL > 2.5:
                    mlp(2, 512)
                fm3 = "skip" if pre else ("data" if ti == NPRE else None)
                if NL > 3:
                    attention(3, 1, 512, 0, fm3, (O["akp"][1], O["avp"][1], None, None) if last else None)
                if pre:
                    continue
                if NL > 3.5:
                    mlp(3, 512)
                store_tile(O["yp"], (ti - NPRE) * 512, 512)
            phase(sp=True)
            NT = DEC_SEQ
            if 'sample' not in DBG:
                S.finish('sp')
                return
            for l in range(2):
                kst = AR.f32(256)
                Rks = ares(f"kst{l}")
                vst = AR.f32(256)
                Rvs = ares(f"vst{l}")
                S.dma("sp", "min", [(kst[:, :], I["ck"][l]), (vst[:, :], I["cv"][l])], writes=[Rks, Rvs])
                pb, rp_ = bank()
                S.group("pe", [TR(pb[0:64, h * 128:(h + 1) * 128], kst[:, h * 64:(h + 1) * 64], ident) for h in range(4)],
                        reads=[Rks, Rc], writes=[rp_])
                S.op("act", lambda e, pb=pb, l=l: e.activation(out=kcar[1][l][:, :], in_=pb[0:64, 0:512], func=AF.Copy),
                     reads=[rp_], writes=[res(f"kcar1{l}")])
                S.op("dve", lambda e, vst=vst, l=l: e.tensor_copy(vcar[1][l][:, :], vst[:, :]), reads=[Rvs], writes=[res(f"vcar1{l}")])
            load_tile(I["xs"], 0, NT)
            if NL > 0:
                attention(0, 0, NT, 1, None, (O["aks"][0], O["avs"][0], I["ck"][0], I["cv"][0]))
            if NL > 0.5:
                mlp(0, NT)
            if NL > 1:
                rwkv(1, NT, 1, False, (O["wkvs"], O["shs"]))
            if NL > 1.5:
                mlp(1, NT)
            if NL > 2:
                conv(2, NT, 1, O["cvs"])
            if NL > 2.5:
                mlp(2, NT)
            if NL > 3:
                attention(3, 1, NT, 1, None, (O["aks"][1], O["avs"][1], I["ck"][1], I["cv"][1]))
            if NL > 3.5:
                mlp(3, NT)
            store_tile(O["ys"], 0, NT)
            S.finish("sp")

        wsched = []
        emit(Sched(sems, record=True))
        S = Sched(sems, record=False)
        emit(S)
        S.replay(block)
        nops = sum(len(v) for v in S.ops.values())
    return nc, nops


_CACHE = {}


def _cols_layout(vecs):
    out = np.zeros((128, NVEC, 8), np.float32)
    for i, v in enumerate(vecs):
        out[:, i, :] = np.asarray(v, np.float32).reshape(8, 128).T
    return np.ascontiguousarray(out.reshape(128, NVEC * 8))


def kernel(x_prompt, x_sample, cache_a_k, cache_a_v, state_b_wkv, state_b_shift, state_c_conv,
           rel_bias_table, norm_g, a_w_qkv, a_w_o, a_sinks,
           b_mu, b_w_rkv, b_w_o, b_w0, b_w1, b_w2, b_a0, b_a1, b_a2, b_g1, b_g2,
           b_k_k, b_k_a, b_r_k, b_ln_w, b_ln_b,
           c_w_in, c_conv_w, c_w_out, mlp_w1, mlp_w2, _full_prefix=False):
    f = lambda a: np.ascontiguousarray(np.asarray(a, np.float32))
    x_prompt = f(x_prompt)
    B, SEQ, _ = x_prompt.shape
    half = SEQ // 2
    NMAIN = half // 512
    NPRE = NMAIN
    key = (NPRE, NMAIN, _full_prefix)
    if key not in _CACHE:
        _CACHE[key] = build_program(NPRE, NMAIN, _full_prefix)
    nc, _ = _CACHE[key]
    consts = make_consts()
    vecs = [norm_g[i][n] for i in range(4) for n in range(4)] + [b_mu[0][i] for i in range(6)] + [
        b_w0[0], b_a0[0], b_k_k[0], b_k_a[0], np.asarray(b_r_k[0]).reshape(-1), b_ln_w[0], b_ln_b[0],
        c_conv_w[0][0], c_conv_w[0][1], c_conv_w[0][2]]
    cols = _cols_layout(vecs)
    shared = dict(
        consts=consts, cols=cols, table=f(rel_bias_table), sinks=f(a_sinks).reshape(1, 32),
        a_w_qkv=f(a_w_qkv), a_w_o=f(a_w_o), b_w_rkv=f(b_w_rkv[0]), b_w_o=f(b_w_o[0]),
        b_w1=f(b_w1[0]), b_w2=f(b_w2[0]), b_a1=f(b_a1[0]), b_a2=f(b_a2[0]), b_g1=f(b_g1[0]), b_g2=f(b_g2[0]),
        c_w_in=f(c_w_in[0]), c_w_out=f(c_w_out[0]), mlp_w1=f(mlp_w1), mlp_w2=f(mlp_w2),
    )
    cache_a_k = f(cache_a_k)
    cache_a_v = f(cache_a_v)
    state_b_wkv = f(state_b_wkv)
    in_maps = []
    for c in range(8):
        b, hf = c // 2, c % 2
        if hf == 0:
            xp = np.concatenate([np.zeros((half, D), np.float32), x_prompt[b, :half]], axis=0)
            pm = np.full((128, 1), NEG, np.float32)
        else:
            xp = x_prompt[b]
            pm = np.zeros((128, 1), np.float32)
        S0 = state_b_wkv[0, c]
        h0 = S0.reshape(8, 2, 64, 64).transpose(1, 3, 0, 2).reshape(128, 512)
        m = dict(shared)
        m.update(
            xp=np.ascontiguousarray(xp), xs=f(x_sample[c]),
            ck=np.ascontiguousarray(cache_a_k[:, c].reshape(2, 128, 256)),
            cv=np.ascontiguousarray(cache_a_v[:, c].reshape(2, 128, 256)),
            h0=np.ascontiguousarray(h0),
            sh0=np.ascontiguousarray(f(state_b_shift)[0, c].reshape(8, 128).T),
            cv0=np.ascontiguousarray(f(state_c_conv)[0, c].reshape(2, 8, 128).transpose(2, 1, 0).reshape(128, 16)),
            pmask=pm)
        in_maps.append(m)
    res = run_bass_kernel_spmd(nc, in_maps, core_ids=list(range(8))).results

    def unH(a):
        return np.asarray(a).reshape(2, 64, 8, 64).transpose(2, 0, 3, 1).reshape(16, 64, 64)

    def uncol(a):
        return np.asarray(a).T.reshape(1024)

    def uncv(a):
        return np.asarray(a).reshape(128, 8, 2).transpose(2, 1, 0).reshape(2, 1024)

    y_prompt = np.stack([np.concatenate([res[2 * b]["yp"], res[2 * b + 1]["yp"]], axis=0) for b in range(B)]).astype(np.float32)
    y_sample = np.stack([res[c]["ys"] for c in range(8)]).astype(np.float32)
    akp = np.stack([np.stack([res[2 * b + 1]["akp"][l].reshape(128, 4, 64) for b in range(B)]) for l in range(2)]).astype(np.float32)
    avp = np.stack([np.stack([res[2 * b + 1]["avp"][l].reshape(128, 4, 64) for b in range(B)]) for l in range(2)]).astype(np.float32)
    aks = np.stack([np.stack([res[c]["aks"][l].reshape(128, 4, 64) for c in range(8)]) for l in range(2)]).astype(np.float32)
    avs = np.stack([np.stack([res[c]["avs"][l].reshape(128, 4, 64) for c in range(8)]) for l in range(2)]).astype(np.float32)
    wkvp = np.stack([unH(res[2 * b + 1]["wkvp"]) for b in range(B)])[None].astype(np.float32)
    wkvs = np.stack([unH(res[c]["wkvs"]) for c in range(8)])[None].astype(np.float32)
    shp = np.stack([uncol(res[2 * b + 1]["shp"]) for b in range(B)])[None].astype(np.float32)
    shs = np.stack([uncol(res[c]["shs"]) for c in range(8)])[None].astype(np.float32)
    cvp = np.stack([uncv(res[2 * b + 1]["cvp"]) for b in range(B)])[None].astype(np.float32)
    cvs = np.stack([uncv(res[c]["cvs"]) for c in range(8)])[None].astype(np.float32)
    return (y_prompt, y_sample, akp, avp, aks, avs, wkvp, wkvs, shp, shs, cvp, cvs)
```

```python
from contextlib import ExitStack
import math
import numpy as np
import concourse.bass as bass
import concourse.mybir as mybir
from concourse.bass_utils import run_bass_kernel_spmd

F32 = mybir.dt.float32
BF16 = mybir.dt.bfloat16
AF = mybir.ActivationFunctionType
ALU = mybir.AluOpType

D = 1024
NH = 16
DH = 64
NKV = 4
DFF = 4096
WIN = 128
DEC_SEQ = 32
NEG = -30000.0
C0 = math.exp(-0.5)
NVEC = 32
V_MU, V_W0, V_A0, V_KK, V_KA, V_RK, V_LNW, V_LNB, V_CW = 16, 22, 23, 24, 25, 26, 27, 28, 29
K_J, K_MP, K_MC, K_ONES, K_BD, K_M1, K_ML, K_SEG, K_OH, K_EPS, K_GEPS, K_ZERO, K_I2, K_NC = (
    0, 384, 512, 640, 768, 896, 1024, 1088, 1600, 1984, 1985, 1986, 1988, 2052)
NSLOT = 5
LOOKAHEAD = 2
SLOT_ELEMS = 4096


class Res:
    __slots__ = ("name", "w", "rs", "excl")

    def __init__(self, name, excl=False):
        self.name = name
        self.w = None
        self.rs = []
        self.excl = excl


class Sched:
    ENG = ("pe", "act", "dve", "pool", "sp")

    def __init__(self, sems, record=False):
        self.record = record
        self.ops = {e: [] for e in self.ENG}
        self.sem = dict(sems)
        self.cnt = {e: 0 for e in self.ENG}
        self.seen = {e: {} for e in self.ENG}
        self.chan_cnt = {}

    def _need(self, eng, dep, same_ok):
        if dep is None:
            return
        key, val = dep
        if key == eng and same_ok:
            return
        if self.seen[eng].get(key, 0) >= val:
            return
        self.seen[eng][key] = val
        sem = self.sem[key]
        self.ops[eng].append(lambda e, sem=sem, val=val: e.wait_ge(sem, val))

    def _deps(self, eng, reads, writes):
        for r in reads:
            self._need(eng, r.w, same_ok=(eng == "pe"))
            if r.excl:
                for rd in r.rs:
                    self._need(eng, rd, same_ok=True)
        for w in writes:
            self._need(eng, w.w, same_ok=True)
            for rd in w.rs:
                self._need(eng, rd, same_ok=True)

    def _mark(self, tag, reads, writes):
        for r in reads:
            r.rs.append(tag)
            if len(r.rs) > 16:
                mx = {}
                for k, v in r.rs:
                    mx[k] = max(mx.get(k, 0), v)
                r.rs = list(mx.items())
        for w in writes:
            w.w = tag
            w.rs = []

    def op(self, eng, fn, reads=(), writes=()):
        if self.record:
            return
        self._deps(eng, reads, writes)
        self.cnt[eng] += 1
        sem = self.sem[eng]
        self.ops[eng].append(lambda e, fn=fn, sem=sem: fn(e).then_inc(sem, 1))
        self._mark((eng, self.cnt[eng]), reads, writes)

    def group(self, eng, fns, reads=(), writes=()):
        if self.record:
            return
        self._deps(eng, reads, writes)
        self.cnt[eng] += 1
        sem = self.sem[eng]
        for fn in fns[:-1]:
            self.ops[eng].append(fn)
        fn = fns[-1]
        self.ops[eng].append(lambda e, fn=fn, sem=sem: fn(e).then_inc(sem, 1))
        self._mark((eng, self.cnt[eng]), reads, writes)

    def dma(self, q, chan, pairs, reads=(), writes=(), slow=False):
        if self.record:
            return
        self._deps(q, reads, writes)
        n = self.chan_cnt.get(chan, 0)
        if n:
            self._need(q, (chan, n), same_ok=False)
        sem = self.sem[chan]
        for (o, i) in pairs:
            if slow:
                self.ops[q].append(lambda e, o=o, i=i, sem=sem: e.dma_start(
                    out=o, in_=i, allow_slow_non_contiguous=True).then_inc(sem, 16))
            else:
                self.ops[q].append(lambda e, o=o, i=i, sem=sem: e.dma_start(out=o, in_=i).then_inc(sem, 16))
        n += 16 * len(pairs)
        self.chan_cnt[chan] = n
        self._mark((chan, n), reads, writes)

    def barrier(self, engines=("pe", "act", "dve", "pool", "sp")):
        if self.record:
            return
        for e in engines:
            for k in self.ENG:
                if k != e and self.cnt[k]:
                    self._need(e, (k, self.cnt[k]), same_ok=False)
            for ch, n in self.chan_cnt.items():
                if not ch.startswith("w"):
                    self._need(e, (ch, n), same_ok=False)

    def finish(self, q="sp"):
        for chan, n in self.chan_cnt.items():
            self._need(q, (chan, n), same_ok=False)
        for e in self.ENG:
            if e != q and self.cnt[e]:
                self._need(q, (e, self.cnt[e]), same_ok=False)

    def replay(self, block):
        ops = self.ops

        @block.tensor
        def _(e):
            for f in ops["pe"]:
                f(e)

        @block.scalar
        def _(e):
            for f in ops["act"]:
                f(e)

        @block.vector
        def _(e):
            for f in ops["dve"]:
                f(e)

        @block.gpsimd
        def _(e):
            for f in ops["pool"]:
                f(e)

        @block.sync
        def _(e):
            for f in ops["sp"]:
                f(e)


def MM(out, lhsT, rhs, start, stop):
    return lambda e: e.matmul(out, lhsT, rhs, start=start, stop=stop)


def TR(out, in_, ident):
    return lambda e: e.transpose(out, in_, ident)


def _rel_bucket_np(rp):
    nb = 16
    ret = (rp > 0).astype(np.int64) * nb
    n = np.abs(rp)
    max_exact = nb // 2
    nf = np.maximum(n, 1).astype(np.float32)
    large = max_exact + (np.log(nf / np.float32(max_exact)) / np.float32(math.log(128 / max_exact))
                         * np.float32(nb - max_exact)).astype(np.int64)
    large = np.minimum(large, nb - 1)
    return ret + np.where(n < max_exact, n, large)


def make_consts():
    c = np.zeros((128, K_NC), np.float32)
    p = np.arange(128)
    for i in range(128):
        c[i, K_J + 128 + i] = 1.0
    k = p[:, None]
    q = p[None, :]
    c[:, K_MP:K_MP + 128] = np.where((k < 64) & (q >= 64), NEG, 0.0)
    c[:, K_MC:K_MC + 128] = np.where((k >= 64) & (q < 64), NEG, 0.0)
    c[:, K_ONES:K_ONES + 128] = 1.0
    c[:, K_BD:K_BD + 128] = ((k // 64) == (q // 64)).astype(np.float32)
    s = (p % 64)[:, None]
    t = np.arange(64)[None, :]
    c[:, K_M1:K_M1 + 64] = (s < t).astype(np.float32)
    c[:, K_M1 + 64:K_M1 + 128] = (s <= t).astype(np.float32)
    c[:, K_ML:K_ML + 64] = (t < s).astype(np.float32)
    seg = np.ones(512, np.float32)
    seg[::64] = 0.0
    c[:, K_SEG:K_SEG + 512] = seg[None, :]
    r = np.arange(384)
    b = _rel_bucket_np(r - 255)
    for i in range(384):
        c[b[i], K_OH + i] = 1.0
    c[:, K_EPS] = 1e-6
    c[:, K_GEPS] = 64 * 1e-5
    c[:, K_ZERO] = 0.0
    c[:, K_ZERO + 1] = 1e-24
    c[:, K_I2:K_I2 + 64] = (s == t).astype(np.float32)
    return c


def build_program(NPRE, NMAIN, full_prefix=False):
    import os
    ATT = int(os.environ.get('KATT', '9'))
    NL = float(os.environ.get('KLAYERS', '9'))
    DBG = set(os.environ.get('KDEBUG', 'attn,mlp,rwkv,conv,sample,bias').split(','))
    nc = bass.Bass("TRN2", target_bir_lowering=False)
    NTOKP = (NPRE + NMAIN) * 512

    def din(name, shape):
        return nc.dram_tensor(name, list(shape), F32, kind="ExternalInput").ap()

    def dout(name, shape):
        return nc.dram_tensor(name, list(shape), F32, kind="ExternalOutput").ap()

    I = dict(
        xp=din("xp", [NTOKP, D]), xs=din("xs", [DEC_SEQ, D]),
        ck=din("ck", [2, 128, 256]), cv=din("cv", [2, 128, 256]),
        h0=din("h0", [128, 8 * 64]), sh0=din("sh0", [128, 8]), cv0=din("cv0", [128, 16]),
        pmask=din("pmask", [128, 1]), consts=din("consts", [128, K_NC]), cols=din("cols", [128, NVEC * 8]),
        table=din("table", [32, 16]), sinks=din("sinks", [1, 32]),
        a_w_qkv=din("a_w_qkv", [2, D, 1536]), a_w_o=din("a_w_o", [2, D, D]),
        b_w_rkv=din("b_w_rkv", [3, D, D]), b_w_o=din("b_w_o", [D, D]),
        b_w1=din("b_w1", [D, 64]), b_w2=din("b_w2", [64, D]), b_a1=din("b_a1", [D, 64]), b_a2=din("b_a2", [64, D]),
        b_g1=din("b_g1", [D, 128]), b_g2=din("b_g2", [128, D]),
        c_w_in=din("c_w_in", [D, 3 * D]), c_w_out=din("c_w_out", [D, D]),
        mlp_w1=din("mlp_w1", [4, D, DFF]), mlp_w2=din("mlp_w2", [4, DFF, D]),
    )
    O = dict(
        yp=dout("yp", [NMAIN * 512, D]), ys=dout("ys", [DEC_SEQ, D]),
        akp=dout("akp", [2, 128, 256]), avp=dout("avp", [2, 128, 256]),
        aks=dout("aks", [2, 128, 256]), avs=dout("avs", [2, 128, 256]),
        wkvp=dout("wkvp", [128, 512]), wkvs=dout("wkvs", [128, 512]),
        shp=dout("shp", [128, 8]), shs=dout("shs", [128, 8]),
        cvp=dout("cvp", [128, 16]), cvs=dout("cvs", [128, 16]),
    )

    BIGW = ["a_w_qkv", "a_w_o", "b_w_rkv", "b_w_o", "c_w_in", "c_w_out", "mlp_w1", "mlp_w2"]
    WB = {n: nc.dram_tensor(n + "_bf", list(I[n].shape), BF16).ap() for n in BIGW}

    with ExitStack() as es:
        def sb(name, shape, dt=F32):
            return es.enter_context(nc.sbuf_tensor(name, list(shape), dt))

        xT = sb("xT", [128, 8 * 512])
        rstd = sb("rstd", [128, 512])
        tmpA = [sb("tmpA0", [128, 512]), sb("tmpA1", [128, 512])]
        slots = [sb(f"slot{i}", [128, SLOT_ELEMS], BF16) for i in range(NSLOT)]
        biasT = [sb("biasP", [128, 16 * 128], BF16), sb("biasC", [128, 16 * 128], BF16)]
        cst = sb("cst", [128, K_NC])
        colv = sb("colv", [128, NVEC * 8])
        omk = sb("omk", [128, 8])
        lw1 = sb("lw1", [128, 8 * 256], BF16)
        lw2 = sb("lw2", [64, 2 * 1024], BF16)
        lg2 = sb("lg2", [128, 1024], BF16)
        ones_bf = sb("ones_bf", [128, 128], BF16)
        bd_bf = sb("bd_bf", [128, 128], BF16)
        stage0_ = sb("stage0", [128, 1024])
        stage = [stage0_, stage0_]
        es_t = sb("es_t", [128, 32])
        tab = sb("tab", [32, 16])
        fsb = sb("fsb", [128, 3 * 16])
        pmk = sb("pmk", [128, 1])
        kvst = sb("kvst", [128, 512])
        pflag = sb("pflag", [128, 1])
        kcar = [[sb(f"kcar{s}{l}", [64, 4 * 128], BF16) for l in range(2)] for s in range(2)]
        vcar = [[sb(f"vcar{s}{l}", [128, 256], BF16) for l in range(2)] for s in range(2)]
        Hst = [sb(f"H{s}", [128, 8 * 64]) for s in range(2)]
        shc = [sb(f"shc{s}", [128, 8]) for s in range(2)]
        ucar = [sb(f"ucar{s}", [128, 16]) for s in range(2)]
        ARENA_F32 = (nc.sbuf_bytes_remaining - 1024) // 4
        arena = sb("arena", [128, ARENA_F32])
        psb = [es.enter_context(nc.psum_tensor(f"psb{i}", [128, 512], F32)) for i in range(8)]

        chans = ([f"ws{i}" for i in range(NSLOT)] + ["xin0", "yout0", "yout1", "min", "minp"] + [f"wc{i}" for i in range(4)])
        sems = {n: es.enter_context(nc.semaphore("s_" + n)) for n in list(Sched.ENG) + chans}
        block = es.enter_context(nc.Block())

        def cs(off, n=1):
            return cst[:, off:off + n]

        ident = cst[:, K_J + 128:K_J + 256]

        def col(v, c):
            return colv[:, v * 8 + c:v * 8 + c + 1]

        def emit(S):
            R = {}

            def res(name):
                if name not in R:
                    R[name] = Res(name)
                return R[name]

            ps_rr = [0]
            RPS = [res(f"ps{i}") for i in range(8)]
            for r_ in RPS:
                r_.excl = True

            def bank():
                i = ps_rr[0] % 7
                ps_rr[0] += 1
                return psb[i], RPS[i]

            class Arena:
                def __init__(self):
                    self.off = 0

                def reset(self):
                    self.off = 0

                def f32(self, n):
                    v = arena[:, self.off:self.off + n]
                    self.off += n
                    assert self.off <= ARENA_F32, ("arena overflow", self.off, ARENA_F32)
                    return v

                def bf16(self, n):
                    assert n % 2 == 0
                    return self.f32(n // 2).bitcast(BF16)

            AR = Arena()
            r_arena_gen = [0]

            def ares(name):
                return res(f"ar{r_arena_gen[0]}_{name}")

            def phase(sp=False):
                S.barrier(("pe", "act", "dve", "pool", "sp") if sp else ("pe", "act", "dve", "pool"))
                AR.reset()
                r_arena_gen[0] += 1

            wstate = {"i": 0, "issued": 0}
            RSLOT = [res(f"slot{i}") for i in range(NSLOT)]

            def wget(dram3d, shape3):
                i = wstate["i"]
                wstate["i"] += 1
                if S.record:
                    wsched.append((dram3d, shape3))
                    return None, None
                while wstate["issued"] < min(len(wsched), i + LOOKAHEAD + 1):
                    n = wstate["issued"]
                    d3, sh = wsched[n]
                    sl = n % NSLOT
                    view = slots[sl][0:sh[0], 0:sh[1] * sh[2]].rearrange("p (a b) -> p a b", a=sh[1])
                    if not S.record:
                        for dep in conv_done:
                            S._need("sp", dep, False)
                    S.dma("sp", f"ws{sl}", [(view, d3)], writes=[RSLOT[sl]])
                    wstate["issued"] += 1
                sl = i % NSLOT
                view = slots[sl][0:shape3[0], 0:shape3[1] * shape3[2]].rearrange("p (a b) -> p a b", a=shape3[1])
                return view, RSLOT[sl]

            def wslab_k(W2d, c0, ncols, kc=8):
                return wget(W2d.rearrange("(k p) o -> p k o", p=128)[:, :, c0:c0 + ncols], (128, kc, ncols))

            conv_done = []
            if not S.record:
                ci = 0
                for n_ in BIGW:
                    src, dst = I[n_], WB[n_]
                    mats = [(src, dst)] if len(src.shape) == 2 else [(src[i_], dst[i_]) for i_ in range(src.shape[0])]
                    for (s2, d2) in mats:
                        rows, cols_ = s2.shape
                        step = 128 if cols_ >= 3072 else 256
                        for r0 in range(0, rows, step):
                            S.dma("pool", f"wc{ci % 4}", [(d2[r0:r0 + step, :], s2[r0:r0 + step, :])])
                            ci += 1
                conv_done = [(f"wc{i_}", S.chan_cnt[f"wc{i_}"]) for i_ in range(4)]
            Rc = res("cst")
            S.dma("sp", "min", [(cst[:, :], I["consts"][:, :])], writes=[Rc])
            Rcol = res("colv")
            S.dma("sp", "min", [(colv[:, :], I["cols"][:, :])], writes=[Rcol])
            Rtab = res("tab")
            S.dma("sp", "min", [(tab[:, :], I["table"][:, :])], writes=[Rtab])
            Res_es = res("es")
            S.dma("sp", "min", [(es_t[:, :], I["sinks"][0:1, :].broadcast_to([128, 32]))], writes=[Res_es])
            Rpm = res("pmk")
            S.dma("sp", "min", [(pmk[:, :], I["pmask"][:, :])], writes=[Rpm])
            Rlw = res("lw")
            S.dma("pool", "minp", [
                (lw1[:, :].rearrange("p (k o) -> p k o", k=8)[:, :, 0:64], I["b_w1"].rearrange("(k p) o -> p k o", p=128)),
                (lw1[:, :].rearrange("p (k o) -> p k o", k=8)[:, :, 64:128], I["b_a1"].rearrange("(k p) o -> p k o", p=128)),
                (lw1[:, :].rearrange("p (k o) -> p k o", k=8)[:, :, 128:256], I["b_g1"].rearrange("(k p) o -> p k o", p=128)),
                (lw2[:, 0:1024], I["b_w2"][:, :]), (lw2[:, 1024:2048], I["b_a2"][:, :]), (lg2[:, :], I["b_g2"][:, :]),
            ], writes=[Rlw])
            S.op("dve", lambda e: e.tensor_scalar(pflag[:, :], pmk[:, :], -1.0 / NEG, 1.0, ALU.mult, ALU.add),
                 reads=[Rpm], writes=[res("pflag")])
            S.op("dve", lambda e: e.tensor_copy(ones_bf[:, :], cs(K_ONES, 128)), reads=[Rc], writes=[res("ones_bf")])
            S.op("dve", lambda e: e.tensor_copy(bd_bf[:, :], cs(K_BD, 128)), reads=[Rc], writes=[res("bd_bf")])
            S.op("dve", lambda e: e.tensor_scalar(omk[:, :], colv[:, V_KA * 8:V_KA * 8 + 8], -1.0, 1.0, ALU.mult, ALU.add),
                 reads=[Rcol], writes=[res("omk")])
            S.op("act", lambda e: e.activation(out=es_t[:, :], in_=es_t[:, :], func=AF.Exp), reads=[Res_es], writes=[Res_es])
            Rf = res("fsb")
            for rc in range(3):
                pb, rp_ = bank()
                S.group("pe", [MM(pb[:, 0:16], cst[0:32, K_OH + rc * 128:K_OH + (rc + 1) * 128], tab[:, :], True, True)],
                        reads=[Rc, Rtab], writes=[rp_])
                S.op("act", lambda e, pb=pb, rc=rc: e.activation(out=fsb[:, rc * 16:(rc + 1) * 16], in_=pb[:, 0:16], func=AF.Copy),
                     reads=[rp_], writes=[Rf])
            Rb = [res("biasP"), res("biasC")]
            for kb in (range(2) if 'bias' in DBG else []):
                bt3 = biasT[kb][:, :].rearrange("p (h q) -> p h q", h=16)
                for q in range(128):
                    off = (127 - q) if kb == 0 else (255 - q)
                    rc0 = off // 128
                    pb, rp_ = bank()
                    fns = []
                    for j, rc in enumerate((rc0, rc0 + 1)):
                        s_ = off - rc * 128
                        fns.append(MM(pb[:, 0:16], cst[:, K_J + s_ + 128:K_J + s_ + 256], fsb[:, rc * 16:(rc + 1) * 16],
                                      j == 0, j == 1))
                    S.group("pe", fns, reads=[Rc, Rf], writes=[rp_])
                    mcol = (K_MP if kb == 0 else K_MC) + q
                    S.op("dve", lambda e, pb=pb, bt3=bt3, q=q, mcol=mcol: e.tensor_scalar(
                        bt3[:, :, q], pb[:, 0:16], cs(mcol), None, ALU.add), reads=[rp_, Rc], writes=[Rb[kb]])

            Rx = res("xT")
            Rrstd = res("rstd")
            Rtmp = [res("tmpA0"), res("tmpA1")]
            tmp_rr = [0]

            def x3(NT):
                return xT[:, :].rearrange("p (c t) -> p c t", c=8)[:, :, 0:NT]

            def rms_stats(src_c, Rsrc, NT, sq, Rsq):
                for c in range(8):
                    S.op("act", lambda e, c=c: e.activation(out=sq[:, c, 0:NT], in_=src_c(c), func=AF.Square),
                         reads=[Rsrc], writes=[Rsq])
                pb, rp_ = bank()
                S.group("pe", [MM(pb[:, 0:NT], ones_bf[:, :], sq[:, c, 0:NT], c == 0, c == 7) for c in range(8)],
                        reads=[Rsq, res("ones_bf")], writes=[rp_])
                S.op("act", lambda e, pb=pb: e.activation(out=rstd[:, 0:NT], in_=pb[:, 0:NT], func=AF.Sqrt,
                                                         bias=cs(K_EPS), scale=1.0 / D), reads=[rp_, Rc], writes=[Rrstd])
                S.op("dve", lambda e: e.reciprocal(rstd[:, 0:NT], rstd[:, 0:NT]), reads=[Rrstd], writes=[Rrstd])

            def norm_apply(gi, dst_c, Rdst, NT):
                xv = x3(NT)
                for c in range(8):
                    S.op("dve", lambda e, c=c: e.scalar_tensor_tensor(dst_c(c), xv[:, c, :], col(gi, c), rstd[:, 0:NT],
                                                                      ALU.mult, ALU.mult),
                         reads=[Rx, Rrstd, Rcol], writes=[Rdst])

            def post_norm_add(gi, m3, Rm, NT, sq, Rsq):
                rms_stats(lambda c: m3[:, c, 0:NT], Rm, NT, sq, Rsq)
                xv = x3(NT)
                for c in range(8):
                    k = tmp_rr[0] % 2
                    tmp_rr[0] += 1
                    S.op("dve", lambda e, c=c, k=k: e.scalar_tensor_tensor(tmpA[k][:, 0:NT], m3[:, c, 0:NT], col(gi, c),
                                                                           rstd[:, 0:NT], ALU.mult, ALU.mult),
                         reads=[Rm, Rrstd, Rcol], writes=[Rtmp[k]])
                    S.op("pool", lambda e, c=c, k=k: e.tensor_tensor(xv[:, c, :], xv[:, c, :], tmpA[k][:, 0:NT], ALU.add),
                         reads=[Rtmp[k], Rx], writes=[Rx])

            def proj_fm(slab_fn, src3, Rsrc, NT, n_out_chunks, evac, kc=8):
                for oc in range(n_out_chunks):
                    lhs_fn, Rw = slab_fn(oc)
                    if lhs_fn is None:
                        continue
                    pb, rp_ = bank()
                    S.group("pe", [MM(pb[:, 0:NT], lhs_fn(k), src3[:, k, 0:NT], k == 0, k == kc - 1) for k in range(kc)],
                            reads=[Rsrc, Rw], writes=[rp_])
                    evac(oc, pb, rp_)

            def mlp(li, NT):
                if 'mlp' not in DBG:
                    return
                phase()
                sq = AR.bf16(8 * 512).rearrange("p (c t) -> p c t", c=8)
                Rsq = ares("sq")
                hT = AR.bf16(8 * 512).rearrange("p (c t) -> p c t", c=8)
                Rh = ares("hT")
                aT = AR.bf16(32 * 512).rearrange("p (c t) -> p c t", c=32)
                Ra = ares("aT")
                mT = AR.f32(8 * 512).rearrange("p (c t) -> p c t", c=8)
                Rm = ares("mT")
                rl = [AR.f32(512), AR.f32(512)]
                Rrl = [ares("rl0"), ares("rl1")]
                rms_stats(lambda c: x3(NT)[:, c, :], Rx, NT, sq, Rsq)
                norm_apply(li * 4 + 2, lambda c: hT[:, c, 0:NT], Rh, NT)
                w1 = WB["mlp_w1"][li]
                w2 = WB["mlp_w2"][li]
                cur = {}

                def slab1(oc):
                    if oc % 4 == 0:
                        cur["v"], cur["r"] = wslab_k(w1, oc * 128, 512)
                    v = cur["v"]
                    return (lambda k, v=v, o=oc % 4: v[:, k, o * 128:(o + 1) * 128]) if v is not None else None, cur["r"]

                def evac1(oc, pb, rp_):
                    k = oc % 2
                    S.op("act", lambda e, pb=pb, k=k: e.activation(out=rl[k][:, 0:NT], in_=pb[:, 0:NT], func=AF.Relu),
                         reads=[rp_], writes=[Rrl[k]])
                    S.op("pool", lambda e, k=k, oc=oc: e.tensor_tensor(aT[:, oc, 0:NT], rl[k][:, 0:NT], rl[k][:, 0:NT], ALU.mult),
                         reads=[Rrl[k]], writes=[Ra])

                proj_fm(slab1, hT, Rh, NT, 32, evac1)

                def evac2(oc, pb, rp_):
                    S.op("act", lambda e, pb=pb, oc=oc: e.activation(out=mT[:, oc, 0:NT], in_=pb[:, 0:NT], func=AF.Copy),
                         reads=[rp_], writes=[Rm])

                w2v = w2.rearrange("(k p) o -> p k o", p=128)
                for half in range(2):
                    banks = [bank() for _ in range(4)]
                    for ks in range(4):
                        v, r = wget(w2v[:, ks * 8:(ks + 1) * 8, half * 512:(half + 1) * 512], (128, 8, 512))
                        if v is None:
                            continue
                        for o in range(4):
                            pb, rp_ = banks[o]
                            S.group("pe", [MM(pb[:, 0:NT], v[:, k, o * 128:(o + 1) * 128], aT[:, ks * 8 + k, 0:NT],
                                              ks == 0 and k == 0, ks == 3 and k == 7) for k in range(8)],
                                    reads=[Ra, r], writes=[rp_])
                    for o in range(4):
                        evac2(half * 4 + o, *banks[o])
                post_norm_add(li * 4 + 3, mT, Rm, NT, sq, Rsq)

            def attention(li, j, NT, st, first_mask, last_out):
                if 'attn' not in DBG:
                    return
                phase()
                QB = min(128, NT)
                NQB = NT // QB
                mflat = AR.f32(8 * 512)
                mT = mflat.rearrange("p (c t) -> p c t", c=8)
                Rm = ares("mT")
                hT = mflat[:, 0:2048].bitcast(BF16).rearrange("p (c t) -> p c t", c=8)
                sq = mflat[:, 2048:4096].bitcast(BF16).rearrange("p (c t) -> p c t", c=8)
                qT = AR.bf16(16 * 512)[0:64, :].rearrange("p (h t) -> p h t", h=16)
                Rq = ares("qT")
                kT = AR.bf16(4 * 640)[0:64, :].rearrange("p (h t) -> p h t", h=4)
                Rk = ares("kT")
                Vb = AR.bf16(5 * 256).rearrange("p (b v) -> p b v", b=5)
                Rv = ares("Vb")
                kvtok = kvst[:, :]
                Rkv = res("kvst")
                sc = [AR.f32(512), AR.f32(512)]
                Rsc = [ares("sc0"), ares("sc1")]
                PT = [AR.bf16(512), AR.bf16(512), AR.bf16(512), AR.bf16(512)]
                RPT = [ares(f"PT{i}") for i in range(4)]
                dn = AR.f32(512)
                Rdn = ares("dn")
                OT = AR.bf16(16 * 512)[0:64, :].rearrange("p (h t) -> p h t", h=16)
                Ro = ares("OT")
                Rkc, Rvc = res(f"kcar{st}{j}"), res(f"vcar{st}{j}")
                kc3 = kcar[st][j][:, :].rearrange("p (h t) -> p h t", h=4)
                S.op("dve", lambda e: e.tensor_copy(kT[:, :, 0:128], kc3), reads=[Rkc], writes=[Rk])
                S.op("dve", lambda e: e.tensor_copy(Vb[:, 0, :], vcar[st][j][:, :]), reads=[Rvc], writes=[Rv])
                rms_stats(lambda c: x3(NT)[:, c, :], Rx, NT, sq, Rm)
                norm_apply(li * 4 + 0, lambda c: hT[:, c, 0:NT], Rm, NT)
                wq = WB["a_w_qkv"][j]
                sl = []
                for g3 in range(3):
                    sl.append(wslab_k(wq, g3 * 512, 512))
                for h in range(20):
                    v, rw = sl[h // 8]
                    pb, rp_ = bank()
                    o = (h % 8) * 64
                    S.group("pe", [MM(pb[0:64, 0:NT], v[:, k, o:o + 64], hT[:, k, 0:NT], k == 0, k == 7) for k in range(8)]
                            if v is not None else [], reads=[Rm, rw], writes=[rp_])
                    if h < 16:
                        S.op("act", lambda e, pb=pb, h=h: e.mul(qT[:, h, 0:NT], pb[0:64, 0:NT], DH ** -0.5), reads=[rp_], writes=[Rq])
                    else:
                        S.op("act", lambda e, pb=pb, h=h: e.activation(out=kT[:, h - 16, 128:128 + NT], in_=pb[0:64, 0:NT],
                                                                        func=AF.Copy), reads=[rp_], writes=[Rk])
                v2, rw2 = sl[2]
                for qb in range(NQB):
                    pb, rp_ = bank()
                    S.group("pe", [MM(pb[0:QB, 0:512], hT[:, k, qb * QB:(qb + 1) * QB], v2[:, k, 0:512], k == 0, k == 7)
                                   for k in range(8)] if v2 is not None else [], reads=[Rm, rw2], writes=[rp_])
                    S.op("dve", lambda e, pb=pb, qb=qb: e.tensor_copy(Vb[0:QB, qb + 1, :], pb[0:QB, 256:512]),
                         reads=[rp_], writes=[Rv])
                    if last_out is not None and qb == NQB - 1 and 'nokvout' not in DBG:
                        S.op("act", lambda e, pb=pb: e.activation(out=kvtok[0:QB, :], in_=pb[0:QB, 0:512], func=AF.Copy),
                             reads=[rp_], writes=[Rkv])
                        ko, vo, cko, cvo = last_out
                        if 'nokvdma' in DBG:
                            pass
                        elif cko is None:
                            S.dma("sp", "yout1", [(ko, kvtok[:, 0:256]), (vo, kvtok[:, 256:512])], reads=[Rkv])
                        else:
                            S.dma("sp", "yout1", [(ko[WIN - QB:WIN, :], kvtok[0:QB, 0:256]), (vo[WIN - QB:WIN, :], kvtok[0:QB, 256:512]),
                                                 (ko[0:WIN - QB, :], cko[QB:WIN, :]), (vo[0:WIN - QB, :], cvo[QB:WIN, :])],
                                  reads=[Rkv])
                es4 = es_t[0:64, j * 16:(j + 1) * 16]
                pt_state = [0]

                def stage_a(qb, kvh):
                    pt_rr = pt_state[0]
                    kbs = []
                    for kb in range(2):
                        if kb == 0 and qb == 0 and first_mask == "skip":
                            continue
                        kbs.append(kb)
                    pts = []
                    for kb in kbs:
                        KB = 128 if kb == 0 else QB
                        c0 = qb * 128 if kb == 0 else 128 + qb * QB
                        pb, rp_ = bank()
                        S.group("pe", [MM(pb[0:KB, 0:4 * QB], kT[:, kvh, c0:c0 + KB], qT[:, 4 * kvh:4 * kvh + 4, qb * QB:(qb + 1) * QB],
                                          True, True)], reads=[Rk, Rq], writes=[rp_])
                        si = pt_rr % 2
                        pi = pt_rr % 4
                        pt_rr += 1
                        b3 = biasT[kb][:, :].rearrange("p (h q) -> p h q", h=16)[0:KB, 4 * kvh:4 * kvh + 4, 0:QB]
                        S.op("dve", lambda e, pb=pb, si=si, KB=KB, b3=b3: e.tensor_tensor(
                            sc[si][0:KB, 0:4 * QB].rearrange("p (g q) -> p g q", g=4),
                            pb[0:KB, 0:4 * QB].rearrange("p (g q) -> p g q", g=4), b3, ALU.add),
                             reads=[rp_, Rb[kb]], writes=[Rsc[si]])
                        if kb == 0 and qb == 0 and first_mask == "data":
                            bias_ap, rbias = pmk[0:KB, 0:1], Rpm
                        else:
                            bias_ap, rbias = cst[0:KB, K_ZERO:K_ZERO + 1], Rc
                        S.op("act", lambda e, si=si, pi=pi, KB=KB, bias_ap=bias_ap: e.activation(
                            out=PT[pi][0:KB, 0:4 * QB], in_=sc[si][0:KB, 0:4 * QB], func=AF.Exp, bias=bias_ap),
                             reads=[Rsc[si], rbias], writes=[RPT[pi]])
                        pts.append((kb, KB, pi))
                    pt_state[0] = pt_rr
                    return pts

                def stage_b(qb, kvh, pts):
                    pbo, rpo = bank()
                    S.group("pe", [MM(pbo[0:64, 0:4 * QB], Vb[0:KB, qb + kb, kvh * 64:(kvh + 1) * 64], PT[pi][0:KB, 0:4 * QB],
                                      n == 0, n == len(pts) - 1) for n, (kb, KB, pi) in enumerate(pts)],
                            reads=[Rv] + [RPT[pi] for (_, _, pi) in pts], writes=[rpo])
                    pbd, rpd = bank()
                    S.group("pe", [MM(pbd[0:64, 0:4 * QB], ones_bf[0:KB, 0:64], PT[pi][0:KB, 0:4 * QB],
                                      n == 0, n == len(pts) - 1) for n, (kb, KB, pi) in enumerate(pts)],
                            reads=[res("ones_bf")] + [RPT[pi] for (_, _, pi) in pts], writes=[rpd])
                    S.op("dve", lambda e, pbd=pbd, kvh=kvh: e.tensor_tensor(
                        dn[0:64, 0:4 * QB].rearrange("p (g q) -> p g q", g=4),
                        pbd[0:64, 0:4 * QB].rearrange("p (g q) -> p g q", g=4),
                        es4[:, 4 * kvh:4 * kvh + 4].unsqueeze(2).to_broadcast([64, 4, QB]), ALU.add),
                         reads=[rpd, Res_es], writes=[Rdn])
                    S.op("dve", lambda e: e.reciprocal(dn[0:64, 0:4 * QB], dn[0:64, 0:4 * QB]), reads=[Rdn], writes=[Rdn])
                    S.op("dve", lambda e, pbo=pbo, kvh=kvh, qb=qb: e.tensor_tensor(
                        OT[:, 4 * kvh:4 * kvh + 4, qb * QB:(qb + 1) * QB],
                        pbo[0:64, 0:4 * QB].rearrange("p (g q) -> p g q", g=4),
                        dn[0:64, 0:4 * QB].rearrange("p (g q) -> p g q", g=4), ALU.mult),
                         reads=[rpo, Rdn], writes=[Ro])

                prev_it = None
                for qb in range(NQB):
                    for kvh in range(NKV):
                        pts_ = stage_a(qb, kvh)
                        if prev_it is not None:
                            stage_b(*prev_it)
                        prev_it = (qb, kvh, pts_)
                if prev_it is not None:
                    stage_b(*prev_it)
                if ATT < 4:
                    return
                if NT >= 128:
                    S.op("dve", lambda e: e.tensor_copy(kc3, kT[:, :, NT:NT + 128]), reads=[Rk], writes=[Rkc])
                    S.op("dve", lambda e: e.tensor_copy(vcar[st][j][:, :], Vb[:, NQB, :]), reads=[Rv], writes=[Rvc])
                wo = WB["a_w_o"][j].rearrange("(h p) o -> p h o", p=64)
                cur = {}

                def slabo(oc):
                    if oc % 2 == 0:
                        cur["v"], cur["r"] = wget(wo[:, :, oc * 128:oc * 128 + 256], (64, 16, 256))
                    v = cur["v"]
                    return (lambda k, v=v, o=oc % 2: v[:, k, o * 128:(o + 1) * 128]) if v is not None else None, cur["r"]

                def evaco(oc, pb, rp_):
                    S.op("act", lambda e, pb=pb, oc=oc: e.activation(out=mT[:, oc, 0:NT], in_=pb[:, 0:NT], func=AF.Copy),
                         reads=[rp_], writes=[Rm])

                proj_fm(slabo, OT, Ro, NT, 8, evaco, kc=16)
                sq2 = AR.bf16(8 * 512).rearrange("p (c t) -> p c t", c=8)
                post_norm_add(li * 4 + 1, mT, Rm, NT, sq2, ares("sq2"))

            def conv(li, NT, st, out_ap):
                if 'conv' not in DBG:
                    return
                phase()
                sq = AR.bf16(8 * 512).rearrange("p (c t) -> p c t", c=8)
                Rsq = ares("sq")
                hT = AR.bf16(8 * 512).rearrange("p (c t) -> p c t", c=8)
                Rh = ares("hT")
                mT = AR.f32(8 * 512).rearrange("p (c t) -> p c t", c=8)
                Rm = ares("mT")
                u = AR.f32(8 * 514).rearrange("p (c t) -> p c t", c=8)
                Ru = ares("u")
                zT = AR.bf16(8 * 512).rearrange("p (c t) -> p c t", c=8)
                Rz = ares("zT")
                cg = [AR.f32(512), AR.f32(512)]
                Rcg = [ares("cg0"), ares("cg1")]
                yv = [AR.f32(512), AR.f32(512)]
                Ry = [ares("yv0"), ares("yv1")]
                Ruc = res(f"ucar{st}")
                uc3 = ucar[st][:, :].rearrange("p (c t) -> p c t", c=8)
                S.op("pool", lambda e: e.tensor_copy(u[:, :, 0:2], uc3), reads=[Ruc], writes=[Ru])
                rms_stats(lambda c: x3(NT)[:, c, :], Rx, NT, sq, Rsq)
                norm_apply(li * 4 + 0, lambda c: hT[:, c, 0:NT], Rh, NT)
                win = WB["c_w_in"]
                sl = {}
                for c in range(8):
                    if c % 4 == 0:
                        for part in range(3):
                            sl[part] = wslab_k(win, part * 1024 + c * 128, 512)
                    pbs = []
                    for part in range(3):
                        v, rw = sl[part]
                        pb, rp_ = bank()
                        o = (c % 4) * 128
                        S.group("pe", [MM(pb[:, 0:NT], v[:, k, o:o + 128], hT[:, k, 0:NT], k == 0, k == 7) for k in range(8)]
                                if v is not None else [], reads=[Rh, rw], writes=[rp_])
                        pbs.append((pb, rp_))
                    k2 = c % 2
                    (pbg, rbg), (pcg, rcg), (phh, rhh) = pbs
                    S.op("act", lambda e, pcg=pcg, k2=k2: e.activation(out=cg[k2][:, 0:NT], in_=pcg[:, 0:NT], func=AF.Copy),
                         reads=[rcg], writes=[Rcg[k2]])
                    S.op("dve", lambda e, phh=phh, k2=k2, c=c: e.tensor_tensor(u[:, c, 2:2 + NT], cg[k2][:, 0:NT], phh[:, 0:NT], ALU.mult),
                         reads=[Rcg[k2], rhh], writes=[Ru])
                    S.op("pool", lambda e, k2=k2, c=c: e.tensor_scalar(yv[k2][:, 0:NT], u[:, c, 0:NT], col(V_CW, c), None, ALU.mult),
                         reads=[Ru, Rcol], writes=[Ry[k2]])
                    S.op("dve", lambda e, k2=k2, c=c: e.scalar_tensor_tensor(yv[k2][:, 0:NT], u[:, c, 1:1 + NT], col(V_CW + 1, c),
                                                                              yv[k2][:, 0:NT], ALU.mult, ALU.add),
                         reads=[Ru, Rcol, Ry[k2]], writes=[Ry[k2]])
                    S.op("dve", lambda e, k2=k2, c=c: e.scalar_tensor_tensor(yv[k2][:, 0:NT], u[:, c, 2:2 + NT], col(V_CW + 2, c),
                                                                              yv[k2][:, 0:NT], ALU.mult, ALU.add),
                         reads=[Ru, Rcol, Ry[k2]], writes=[Ry[k2]])
                    S.op("dve", lambda e, pbg=pbg, k2=k2, c=c: e.tensor_tensor(zT[:, c, 0:NT], yv[k2][:, 0:NT], pbg[:, 0:NT], ALU.mult),
                         reads=[Ry[k2], rbg], writes=[Rz])
                S.op("pool", lambda e: e.tensor_copy(uc3, u[:, :, NT:NT + 2]), reads=[Ru], writes=[Ruc])
                if out_ap is not None:
                    S.dma("sp", "yout1", [(out_ap[:, :], ucar[st][:, :])], reads=[Ruc])
                wout = WB["c_w_out"]
                cur = {}

                def slabo(oc):
                    if oc % 4 == 0:
                        cur["v"], cur["r"] = wslab_k(wout, oc * 128, 512)
                    v = cur["v"]
                    return (lambda k, v=v, o=oc % 4: v[:, k, o * 128:(o + 1) * 128]) if v is not None else None, cur["r"]

                def evaco(oc, pb, rp_):
                    S.op("act", lambda e, pb=pb, oc=oc: e.activation(out=mT[:, oc, 0:NT], in_=pb[:, 0:NT], func=AF.Copy),
                         reads=[rp_], writes=[Rm])

                proj_fm(slabo, zT, Rz, NT, 8, evaco)
                post_norm_add(li * 4 + 1, mT, Rm, NT, sq, Rsq)

            def rwkv(li, NT, st, state_only, outs):
                if 'rwkv' not in DBG:
                    return
                phase()
                C = min(64, NT)
                NTC = NT // C
                regA = AR.f32(8 * 513 + 8 * 512)
                h32 = regA[:, 0:8 * 513].rearrange("p (c t) -> p c t", c=8)
                Rh32 = ares("h32")
                xxf = regA[:, 8 * 513:8 * 513 + 8 * 512]
                xx = xxf.rearrange("p (c t) -> p c t", c=8)
                Rxx = ares("xx")
                xm = [AR.bf16(8 * 512).rearrange("p (c t) -> p c t", c=8) for _ in range(3)]
                Rxm = [ares(f"xm{i}") for i in range(3)]
                tw = AR.bf16(512)
                ta = AR.bf16(512)
                tg = AR.bf16(512)
                Rlo = ares("lora")
                zT = AR.bf16(8 * 512).rearrange("p (c t) -> p c t", c=8)
                Rz = ares("zT")
                Rsh = res(f"shc{st}")
                RH = res(f"H{st}")
                H3 = Hst[st][:, :].rearrange("p (c j) -> p c j", c=8)
                sq = xxf[:, 0:2048].bitcast(BF16).rearrange("p (c t) -> p c t", c=8)
                rms_stats(lambda c: x3(NT)[:, c, :], Rx, NT, sq, Rxx)
                S.op("pool", lambda e: e.tensor_copy(h32[:, :, 0:1], shc[st][:, :].unsqueeze(2)), reads=[Rsh], writes=[Rh32])
                norm_apply(li * 4 + 0, lambda c: h32[:, c, 1:1 + NT], Rh32, NT)
                S.op("pool", lambda e: e.tensor_copy(shc[st][:, :].unsqueeze(2), h32[:, :, NT:NT + 1]), reads=[Rh32], writes=[Rsh])
                if outs is not None:
                    S.dma("sp", "yout1", [(outs[1][:, :], shc[st][:, :])], reads=[Rsh])
                for c in range(8):
                    S.op("pool", lambda e, c=c: e.tensor_tensor(xx[:, c, 0:NT], h32[:, c, 0:NT], h32[:, c, 1:1 + NT], ALU.subtract),
                         reads=[Rh32], writes=[Rxx])

                def mix(mi, dst, Rdst):
                    for c in range(8):
                        S.op("dve", lambda e, c=c: e.scalar_tensor_tensor(
                            dst[:, c, 0:NT], xx[:, c, 0:NT], col(V_MU + mi, c), h32[:, c, 1:1 + NT], ALU.mult, ALU.add),
                             reads=[Rxx, Rh32, Rcol], writes=[Rdst])

                lw1v = lw1[:, :].rearrange("p (k o) -> p k o", k=8)
                mix(1, xm[0], Rxm[0])
                mix(4, xm[1], Rxm[1])
                mix(5, xm[2], Rxm[2])
                for (src, Rs, o0, M, dst, fn) in ((xm[0], Rxm[0], 0, 64, tw, AF.Tanh), (xm[1], Rxm[1], 64, 64, ta, AF.Copy),
                                                  (xm[2], Rxm[2], 128, 128, tg, AF.Sigmoid)):
                    pb, rp_ = bank()
                    S.group("pe", [MM(pb[0:M, 0:NT], lw1v[:, k, o0:o0 + M], src[:, k, 0:NT], k == 0, k == 7) for k in range(8)],
                            reads=[Rs, Rlw], writes=[rp_])
                    S.op("act", lambda e, pb=pb, M=M, dst=dst, fn=fn: e.activation(out=dst[0:M, 0:NT], in_=pb[0:M, 0:NT], func=fn),
                         reads=[rp_], writes=[Rlo])
                mix(0, xm[0], Rxm[0])
                mix(2, xm[1], Rxm[1])
                mix(3, xm[2], Rxm[2])
                S.barrier()
                sub = [0]

                def T2(n=512):
                    v = regA[:, sub[0]:sub[0] + n]
                    sub[0] += n
                    assert sub[0] <= 8 * 513 + 8 * 512
                    return v
                rT, kTt, aTt, sg, csum, kk, t2, bon, vv = [T2() for _ in range(9)]
                t3 = aTt
                arB = T2(1024).rearrange("p (a t) -> p a t", a=2)
                bb, kbar, btl, ktl = [T2() for _ in range(4)]
                sqb = T2(256).bitcast(BF16)
                gam = T2(8)
                Lm = AR.f32(512).rearrange("p (n s) -> p n s", n=8)
                MA = [AR.f32(1024).rearrange("p (n s) -> p n s", n=8) for _ in range(2)]
                Xp = [AR.f32(512).rearrange("p (n s) -> p n s", n=8) for _ in range(2)]
                Mp = [AR.f32(512).rearrange("p (n s) -> p n s", n=8) for _ in range(2)]
                Pp = [AR.f32(512).rearrange("p (n s) -> p n s", n=8) for _ in range(2)]
                Btk = AR.f32(512).rearrange("p (n s) -> p n s", n=8)
                Ktk = AR.f32(512).rearrange("p (n s) -> p n s", n=8)
                Vtk = AR.f32(512).rearrange("p (n s) -> p n s", n=8)
                Wb = AR.f32(64)
                Ub = AR.f32(64)
                ysb = AR.f32(512)
                Halt = AR.f32(64)
                RT = {n: ares("rw_" + n) for n in ("r", "k", "a", "sg", "cs", "kk", "t2", "t3", "bon", "vv", "ar", "bb", "kb", "bt",
                                                  "kt", "sqb", "gam", "L", "MA0", "MA1", "X0", "X1", "M0", "M1", "P0", "P1", "Bt",
                                                  "Kt", "Vt", "Wb", "Ub", "ysb", "Halt")}
                RT["t3"] = RT["a"]
                wr = WB["b_w_rkv"]
                sl = {}
                Rbd = res("bd_bf")
                for c in range(8):
                    if c % 4 == 0:
                        for part in range(3):
                            sl[part] = wslab_k(wr[part], c * 128, 512)
                    o = (c % 4) * 128
                    pr = []
                    for part in range(3):
                        if part == 0 and state_only:
                            pr.append((None, None))
                            continue
                        v, rw = sl[part]
                        pb, rp_ = bank()
                        S.group("pe", [MM(pb[:, 0:NT], v[:, k, o:o + 128], xm[part][:, k, 0:NT], k == 0, k == 7) for k in range(8)]
                                if v is not None else [], reads=[Rxm[part], rw], writes=[rp_])
                        pr.append((pb, rp_))
                    (pr_r, rr_r), (pr_k, rr_k), (pr_v, rr_v) = pr
                    if not state_only:
                        S.op("act", lambda e, p=pr_r: e.activation(out=rT[:, 0:NT], in_=p[:, 0:NT], func=AF.Copy), reads=[rr_r], writes=[RT["r"]])
                    S.op("act", lambda e, p=pr_k: e.activation(out=kTt[:, 0:NT], in_=p[:, 0:NT], func=AF.Copy), reads=[rr_k], writes=[RT["k"]])
                    S.op("act", lambda e, p=pr_v: e.activation(out=vv[:, 0:NT], in_=p[:, 0:NT], func=AF.Copy), reads=[rr_v], writes=[RT["vv"]])
                    pb, rp_ = bank()
                    S.group("pe", [MM(pb[:, 0:NT], lw2[:, c * 128:(c + 1) * 128], tw[0:64, 0:NT], True, True)], reads=[Rlo, Rlw], writes=[rp_])
                    S.op("act", lambda e, pb=pb, c=c: e.activation(out=sg[:, 0:NT], in_=pb[:, 0:NT], func=AF.Sigmoid, bias=col(V_W0, c)),
                         reads=[rp_, Rcol], writes=[RT["sg"]])
                    pb, rp_ = bank()
                    S.group("pe", [MM(pb[:, 0:NT], lw2[:, 1024 + c * 128:1024 + (c + 1) * 128], ta[0:64, 0:NT], True, True)], reads=[Rlo, Rlw], writes=[rp_])
                    S.op("act", lambda e, pb=pb, c=c: e.activation(out=aTt[:, 0:NT], in_=pb[:, 0:NT], func=AF.Sigmoid, bias=col(V_A0, c)),
                         reads=[rp_, Rcol], writes=[RT["a"]])
                    S.op("dve", lambda e, c=c: e.tensor_scalar(kk[:, 0:NT], kTt[:, 0:NT], col(V_KK, c), None, ALU.mult),
                         reads=[RT["k"], Rcol], writes=[RT["kk"]])
                    S.op("act", lambda e: e.activation(out=sqb[:, 0:NT], in_=kk[:, 0:NT], func=AF.Square), reads=[RT["kk"]], writes=[RT["sqb"]])
                    pb, rp_ = bank()
                    S.group("pe", [MM(pb[:, 0:NT], bd_bf[:, :], sqb[:, 0:NT], True, True)], reads=[RT["sqb"], Rbd], writes=[rp_])
                    S.op("act", lambda e, pb=pb: e.activation(out=t2[:, 0:NT], in_=pb[:, 0:NT], func=AF.Sqrt, bias=cs(K_ZERO + 1)),
                         reads=[rp_, Rc], writes=[RT["t2"]])
                    S.op("dve", lambda e: e.reciprocal(t2[:, 0:NT], t2[:, 0:NT]), reads=[RT["t2"]], writes=[RT["t2"]])
                    S.op("dve", lambda e: e.tensor_tensor(kk[:, 0:NT], kk[:, 0:NT], t2[:, 0:NT], ALU.mult), reads=[RT["kk"], RT["t2"]], writes=[RT["kk"]])
                    S.op("pool", lambda e, c=c: e.tensor_scalar(t2[:, 0:NT], aTt[:, 0:NT], col(V_KA, c), omk[:, c:c + 1], ALU.mult, ALU.add),
                         reads=[RT["a"], Rcol, res("omk"), RT["t2"]], writes=[RT["t2"]])
                    S.op("pool", lambda e: e.tensor_tensor(kTt[:, 0:NT], kTt[:, 0:NT], t2[:, 0:NT], ALU.mult), reads=[RT["k"], RT["t2"]], writes=[RT["k"]])
                    S.op("pool", lambda e: e.tensor_tensor(t3[:, 0:NT], kk[:, 0:NT], aTt[:, 0:NT], ALU.mult), reads=[RT["kk"], RT["a"]], writes=[RT["t3"]])
                    if not state_only:
                        S.op("dve", lambda e, c=c: e.scalar_tensor_tensor(sqb[:, 0:NT], rT[:, 0:NT], col(V_RK, c), kTt[:, 0:NT], ALU.mult, ALU.mult),
                             reads=[RT["r"], RT["k"], Rcol, RT["sqb"]], writes=[RT["sqb"]])
                        pb, rp_ = bank()
                        S.group("pe", [MM(pb[:, 0:NT], bd_bf[:, :], sqb[:, 0:NT], True, True)], reads=[RT["sqb"], Rbd], writes=[rp_])
                        S.op("dve", lambda e, pb=pb: e.tensor_tensor(bon[:, 0:NT], pb[:, 0:NT], vv[:, 0:NT], ALU.mult),
                             reads=[rp_, RT["vv"]], writes=[RT["bon"]])
                    S.op("dve", lambda e: e.tensor_tensor_scan(csum[:, 0:NT], cst[:, K_SEG:K_SEG + NT], sg[:, 0:NT], 0.0, ALU.mult, ALU.add),
                         reads=[RT["sg"], Rc], writes=[RT["cs"]])
                    cs3 = csum[:, 0:NT].rearrange("p (n t) -> p n t", n=NTC)
                    S.op("act", lambda e: e.activation(out=t2[:, 0:NT], in_=csum[:, 0:NT], func=AF.Exp, scale=-C0), reads=[RT["cs"], RT["t2"]], writes=[RT["t2"]])
                    S.op("dve", lambda e: e.tensor_copy(gam[:, 0:NTC].unsqueeze(2), t2[:, 0:NT].rearrange("p (n t) -> p n t", n=NTC)[:, :, C - 1:C]),
                         reads=[RT["t2"]], writes=[RT["gam"]])
                    if not state_only:
                        S.op("dve", lambda e: e.tensor_tensor(arB[:, 1, 0:NT], rT[:, 0:NT], t2[:, 0:NT], ALU.mult), reads=[RT["r"], RT["t2"]], writes=[RT["ar"]])
                    S.op("pool", lambda e: e.tensor_tensor(t2[:, 0:NT], csum[:, 0:NT], sg[:, 0:NT], ALU.subtract), reads=[RT["cs"], RT["sg"], RT["t2"]], writes=[RT["t2"]])
                    S.op("act", lambda e: e.activation(out=t2[:, 0:NT], in_=t2[:, 0:NT], func=AF.Exp, scale=-C0), reads=[RT["t2"]], writes=[RT["t2"]])
                    S.op("dve", lambda e: e.scalar_tensor_tensor(arB[:, 0, 0:NT], kk[:, 0:NT], -1.0, t2[:, 0:NT], ALU.mult, ALU.mult),
                         reads=[RT["kk"], RT["t2"]], writes=[RT["ar"]])
                    S.op("act", lambda e: e.activation(out=t2[:, 0:NT], in_=csum[:, 0:NT], func=AF.Exp, scale=C0), reads=[RT["cs"], RT["t2"]], writes=[RT["t2"]])
                    S.op("dve", lambda e: e.tensor_tensor(bb[:, 0:NT], t3[:, 0:NT], t2[:, 0:NT], ALU.mult), reads=[RT["t3"], RT["t2"]], writes=[RT["bb"]])
                    S.op("pool", lambda e: e.tensor_tensor(kbar[:, 0:NT], kTt[:, 0:NT], t2[:, 0:NT], ALU.mult), reads=[RT["k"], RT["t2"]], writes=[RT["kb"]])
                    S.op("dve", lambda e: e.tensor_tensor(t2[:, 0:NT].rearrange("p (n t) -> p n t", n=NTC),
                                                          cs3[:, :, C - 1:C].to_broadcast([128, NTC, C]), cs3, ALU.subtract),
                         reads=[RT["cs"], RT["t2"]], writes=[RT["t2"]])
                    S.op("act", lambda e: e.activation(out=t2[:, 0:NT], in_=t2[:, 0:NT], func=AF.Exp, scale=-C0), reads=[RT["t2"]], writes=[RT["t2"]])
                    S.op("dve", lambda e: e.tensor_tensor(btl[:, 0:NT], t3[:, 0:NT], t2[:, 0:NT], ALU.mult), reads=[RT["t3"], RT["t2"]], writes=[RT["bt"]])
                    S.op("pool", lambda e: e.tensor_tensor(ktl[:, 0:NT], kTt[:, 0:NT], t2[:, 0:NT], ALU.mult), reads=[RT["k"], RT["t2"]], writes=[RT["kt"]])

                    def hs(hh):
                        return slice(64 * hh, 64 * hh + 64)

                    def tcs(n):
                        return slice(n * C, (n + 1) * C)

                    def ps_(hh):
                        return slice(64 * hh, 64 * hh + C)
                    pb, rp_ = bank()
                    S.group("pe", [MM(pb[ps_(hh), n * 64:n * 64 + C], arB[hs(hh), 0, tcs(n)], bb[hs(hh), tcs(n)], True, True)
                                   for n in range(NTC) for hh in range(2)], reads=[RT["ar"], RT["bb"]], writes=[rp_])
                    S.op("dve", lambda e, pb=pb: e.tensor_tensor(Lm[:, 0:NTC, 0:C], pb[:, 0:NTC * 64].rearrange("p (n s) -> p n s", n=NTC)[:, :, 0:C],
                                                                 cst[:, K_ML:K_ML + C].unsqueeze(1).to_broadcast([128, NTC, C]), ALU.mult),
                         reads=[rp_, Rc], writes=[RT["L"]])
                    nA = 1 if state_only else 2
                    for mi, (lhs, Rl) in enumerate(((bb, RT["bb"]), (kbar, RT["kb"]))):
                        for half in range(2):
                            n0 = half * (NTC // 2 if NTC > 1 else 1)
                            n1 = NTC if (half == 1 or NTC == 1) else NTC // 2
                            if n0 >= n1:
                                continue
                            pb, rp_ = bank()
                            S.group("pe", [MM(pb[ps_(hh), (n - n0) * 128:(n - n0 + 1) * 128].rearrange("p (a t) -> p a t", a=2)[:, 0:nA, 0:C],
                                              lhs[hs(hh), tcs(n)], arB[hs(hh), 0:nA, tcs(n)], True, True)
                                           for n in range(n0, n1) for hh in range(2)], reads=[Rl, RT["ar"]], writes=[rp_])
                            for a_ in range(nA):
                                S.op("dve", lambda e, pb=pb, mi=mi, n0=n0, n1=n1, a_=a_: e.tensor_tensor(
                                    MA[mi][:, n0:n1, a_ * 64:a_ * 64 + C],
                                    pb[:, 0:(n1 - n0) * 128].rearrange("p (n x) -> p n x", x=128)[:, :, a_ * 64:a_ * 64 + C],
                                    cst[:, K_M1 + a_ * 64:K_M1 + a_ * 64 + C].unsqueeze(1).to_broadcast([128, n1 - n0, C]),
                                    ALU.mult), reads=[rp_, Rc], writes=[RT[f"MA{mi}"]])
                    for (src, Rs_, dst, Rd_) in ((btl, RT["bt"], Btk, RT["Bt"]), (ktl, RT["kt"], Ktk, RT["Kt"]), (vv, RT["vv"], Vtk, RT["Vt"])):
                        pb, rp_ = bank()
                        S.group("pe", [MM(pb[64 * hh:64 * hh + C, n * 64:(n + 1) * 64], src[hs(hh), tcs(n)], ident[hs(hh), hs(hh)], True, True)
                                       for n in range(NTC) for hh in range(2)], reads=[Rs_, Rc], writes=[rp_])
                        if C == 64:
                            S.op("act", lambda e, pb=pb, dst=dst: e.activation(out=dst[:, 0:NTC, :], in_=pb[:, 0:NTC * 64].rearrange("p (n s) -> p n s", n=NTC),
                                                                              func=AF.Copy), reads=[rp_], writes=[Rd_])
                        else:
                            for hh in range(2):
                                S.op("act", lambda e, pb=pb, dst=dst, hh=hh: e.activation(out=dst[64 * hh:64 * hh + C, 0, :], in_=pb[64 * hh:64 * hh + C, 0:64],
                                                                                         func=AF.Copy), reads=[rp_], writes=[Rd_])
                    S.op("pool", lambda e: e.tensor_tensor(Pp[0][:, 0:NTC, 0:C], MA[0][:, 0:NTC, 0:C],
                                                          cst[:, K_I2:K_I2 + C].unsqueeze(1).to_broadcast([128, NTC, C]), ALU.add),
                         reads=[RT["MA0"], Rc], writes=[RT["P0"]])
                    Xc, RXc = Lm, RT["L"]
                    Mc, RMc = MA[0], RT["MA0"]
                    pcur = 0
                    nlev = 5 if C == 64 else 4
                    for m in range(nlev):
                        Xn, RXn = Xp[m % 2], RT[f"X{m % 2}"]
                        Mn, RMn = Mp[m % 2], RT[f"M{m % 2}"]
                        last = (m == nlev - 1)
                        pbx, rpx = bank()
                        S.group("pe", [MM(pbx[ps_(hh), n * 64:n * 64 + C], Mc[ps_(hh), n, 0:C], Xc[ps_(hh), n, 0:C], True, True)
                                       for n in range(NTC) for hh in range(2)], reads=[RXc, RMc], writes=[rpx])
                        S.op("act", lambda e, pbx=pbx, Xn=Xn: e.activation(out=Xn[:, 0:NTC, 0:C], in_=pbx[:, 0:NTC * 64].rearrange("p (n s) -> p n s", n=NTC)[:, :, 0:C],
                                                                          func=AF.Copy), reads=[rpx], writes=[RXn])
                        if not last:
                            pbm, rpm = bank()
                            S.group("pe", [MM(pbm[ps_(hh), n * 64:n * 64 + C], Xc[ps_(hh), n, 0:C], Mc[ps_(hh), n, 0:C], True, True)
                                           for n in range(NTC) for hh in range(2)], reads=[RXc, RMc], writes=[rpm])
                            S.op("dve", lambda e, pbm=pbm, Mn=Mn: e.tensor_copy(Mn[:, 0:NTC, 0:C], pbm[:, 0:NTC * 64].rearrange("p (n s) -> p n s", n=NTC)[:, :, 0:C]),
                                 reads=[rpm], writes=[RMn])
                        pbp, rpp = bank()
                        S.group("pe", [MM(pbp[ps_(hh), n * 64:n * 64 + C], Xn[ps_(hh), n, 0:C], Pp[pcur][ps_(hh), n, 0:C], True, True)
                                       for n in range(NTC) for hh in range(2)], reads=[RXn, RT[f"P{pcur}"]], writes=[rpp])
                        S.op("dve", lambda e, pbp=pbp, pcur=pcur: e.tensor_tensor(
                            Pp[1 - pcur][:, 0:NTC, 0:C], pbp[:, 0:NTC * 64].rearrange("p (n s) -> p n s", n=NTC)[:, :, 0:C],
                            Pp[pcur][:, 0:NTC, 0:C], ALU.add), reads=[rpp, RT[f"P{pcur}"]], writes=[RT[f"P{1 - pcur}"]])
                        pcur = 1 - pcur
                        Xc, RXc = Xn, RXn
                        Mc, RMc = Mn, RMn
                    PT_, RPT_ = Pp[pcur], RT[f"P{pcur}"]
                    if not state_only:
                        pby, rpy = psb[7], RPS[7]
                    for n in range(NTC):
                        Ho, RHo = (H3[:, c, :], RH) if n % 2 == 0 else (Halt[:, :], RT["Halt"])
                        Hn, RHn = (Halt[:, :], RT["Halt"]) if n % 2 == 0 else (H3[:, c, :], RH)
                        pw, rpw = bank()
                        fns = []
                        for hh in range(2):
                            fns.append(MM(pw[ps_(hh), 0:64], arB[hs(hh), 0, tcs(n)], Ho[hs(hh), :], True, False))
                            fns.append(MM(pw[ps_(hh), 0:64], MA[1][ps_(hh), n, 0:C], Vtk[ps_(hh), n, :], False, True))
                        S.group("pe", fns, reads=[RT["ar"], RHo, RT["MA1"], RT["Vt"]], writes=[rpw])
                        if C == 64:
                            S.op("act", lambda e, pw=pw: e.activation(out=Wb[:, 0:64], in_=pw[:, 0:64], func=AF.Copy), reads=[rpw], writes=[RT["Wb"]])
                        else:
                            for hh in range(2):
                                S.op("act", lambda e, pw=pw, hh=hh: e.activation(out=Wb[ps_(hh), 0:64], in_=pw[ps_(hh), 0:64], func=AF.Copy),
                                     reads=[rpw], writes=[RT["Wb"]])
                        pu, rpu = bank()
                        S.group("pe", [MM(pu[ps_(hh), 0:64], PT_[ps_(hh), n, 0:C], Wb[ps_(hh), 0:64], True, True) for hh in range(2)],
                                reads=[RPT_, RT["Wb"]], writes=[rpu])
                        if C == 64:
                            S.op("dve", lambda e, pu=pu: e.tensor_copy(Ub[:, 0:64], pu[:, 0:64]), reads=[rpu], writes=[RT["Ub"]])
                        else:
                            for hh in range(2):
                                S.op("dve", lambda e, pu=pu, hh=hh: e.tensor_copy(Ub[ps_(hh), 0:64], pu[ps_(hh), 0:64]), reads=[rpu], writes=[RT["Ub"]])
                        ph, rph = bank()
                        fns = []
                        for hh in range(2):
                            fns.append(MM(ph[hs(hh), 0:64], Btk[ps_(hh), n, :], Ub[ps_(hh), 0:64], True, False))
                            fns.append(MM(ph[hs(hh), 0:64], Ktk[ps_(hh), n, :], Vtk[ps_(hh), n, :], False, True))
                        S.group("pe", fns, reads=[RT["Bt"], RT["Kt"], RT["Ub"], RT["Vt"]], writes=[rph])
                        if not state_only:
                            fns = []
                            for hh in range(2):
                                oy = pby[hs(hh), n * C:(n + 1) * C]
                                fns.append(MM(oy, Ho[hs(hh), :], arB[hs(hh), 1, tcs(n)], True, False))
                                fns.append(MM(oy, Ub[ps_(hh), 0:64], MA[0][ps_(hh), n, 64:64 + C], False, False))
                                fns.append(MM(oy, Vtk[ps_(hh), n, :], MA[1][ps_(hh), n, 64:64 + C], False, True))
                            S.group("pe", fns, reads=[RHo, RT["ar"], RT["Ub"], RT["MA0"], RT["MA1"], RT["Vt"]], writes=[rpy])
                        S.op("dve", lambda e, ph=ph, n=n, Ho=Ho, Hn=Hn: e.scalar_tensor_tensor(Hn, Ho, gam[:, n:n + 1], ph[:, 0:64], ALU.mult, ALU.add),
                             reads=[rph, RT["gam"], RHo], writes=[RHn])
                    if NTC % 2 == 1:
                        S.op("dve", lambda e, c=c: e.tensor_copy(H3[:, c, :], Halt[:, :]), reads=[RT["Halt"]], writes=[RH])
                    if not state_only:
                        S.op("act", lambda e, pby=pby: e.activation(out=ysb[:, 0:NT], in_=pby[:, 0:NT], func=AF.Copy), reads=[rpy], writes=[RT["ysb"]])
                        S.op("dve", lambda e: e.tensor_copy(sqb[:, 0:NT], ysb[:, 0:NT]), reads=[RT["ysb"], RT["sqb"]], writes=[RT["sqb"]])
                        pm_, rpm_ = bank()
                        S.group("pe", [MM(pm_[:, 0:NT], bd_bf[:, :], sqb[:, 0:NT], True, True)], reads=[RT["sqb"], Rbd], writes=[rpm_])
                        S.op("dve", lambda e, pm_=pm_: e.scalar_tensor_tensor(ysb[:, 0:NT], pm_[:, 0:NT], -1.0 / 64, ysb[:, 0:NT], ALU.mult, ALU.add),
                             reads=[rpm_, RT["ysb"]], writes=[RT["ysb"]])
                        S.op("act", lambda e: e.activation(out=sqb[:, 0:NT], in_=ysb[:, 0:NT], func=AF.Square), reads=[RT["ysb"], RT["sqb"]], writes=[RT["sqb"]])
                        pv_, rpv_ = bank()
                        S.group("pe", [MM(pv_[:, 0:NT], bd_bf[:, :], sqb[:, 0:NT], True, True)], reads=[RT["sqb"], Rbd], writes=[rpv_])
                        S.op("act", lambda e, pv_=pv_: e.activation(out=t2[:, 0:NT], in_=pv_[:, 0:NT], func=AF.Sqrt, bias=cs(K_GEPS), scale=1.0 / 64),
                             reads=[rpv_, Rc, RT["t2"]], writes=[RT["t2"]])
                        S.op("dve", lambda e: e.reciprocal(t2[:, 0:NT], t2[:, 0:NT]), reads=[RT["t2"]], writes=[RT["t2"]])
                        S.op("dve", lambda e: e.tensor_tensor(ysb[:, 0:NT], ysb[:, 0:NT], t2[:, 0:NT], ALU.mult), reads=[RT["ysb"], RT["t2"]], writes=[RT["ysb"]])
                        S.op("dve", lambda e, c=c: e.tensor_scalar(ysb[:, 0:NT], ysb[:, 0:NT], col(V_LNW, c), col(V_LNB, c), ALU.mult, ALU.add),
                             reads=[RT["ysb"], Rcol], writes=[RT["ysb"]])
                        S.op("pool", lambda e: e.tensor_tensor(ysb[:, 0:NT], ysb[:, 0:NT], bon[:, 0:NT], ALU.add), reads=[RT["ysb"], RT["bon"]], writes=[RT["ysb"]])
                        pg, rpg = bank()
                        S.group("pe", [MM(pg[:, 0:NT], lg2[:, c * 128:(c + 1) * 128], tg[:, 0:NT], True, True)], reads=[Rlo, Rlw], writes=[rpg])
                        S.op("dve", lambda e, pg=pg, c=c: e.tensor_tensor(zT[:, c, 0:NT], ysb[:, 0:NT], pg[:, 0:NT], ALU.mult),
                             reads=[rpg, RT["ysb"]], writes=[Rz])
                if outs is not None:
                    S.dma("sp", "yout1", [(outs[0][:, :], Hst[st][:, :])], reads=[RH])
                if state_only:
                    return
                S.barrier()
                mT3 = h32[:, :, 0:512]
                Rm = Rh32
                wo = WB["b_w_o"]
                cur = {}

                def slabo(oc):
                    if oc % 4 == 0:
                        cur["v"], cur["r"] = wslab_k(wo, oc * 128, 512)
                    v = cur["v"]
                    return (lambda k, v=v, o=oc % 4: v[:, k, o * 128:(o + 1) * 128]) if v is not None else None, cur["r"]

                def evaco(oc, pb, rp_):
                    S.op("act", lambda e, pb=pb, oc=oc: e.activation(out=mT3[:, oc, 0:NT], in_=pb[:, 0:NT], func=AF.Copy),
                         reads=[rp_], writes=[Rm])

                proj_fm(slabo, zT, Rz, NT, 8, evaco)
                post_norm_add(li * 4 + 1, mT3, Rm, NT, sq, Rxx)

            Rst = [res("stage0"), res("stage0")]
            st_rr = [0]

            def load_tile(src2d, t0, NT):
                QB = min(128, NT)
                xv = xT[:, :].rearrange("p (c t) -> p c t", c=8)
                for b in range(NT // QB):
                    k = st_rr[0] % 2
                    st_rr[0] += 1
                    S.dma("sp", "xin0", [(stage[k][0:QB, :], src2d[t0 + b * QB:t0 + (b + 1) * QB, :])], writes=[Rst[k]])
                    for half in range(2):
                        pb, rp_ = bank()
                        S.group("pe", [TR(pb[:, cc * QB:(cc + 1) * QB], stage[k][0:QB, (half * 4 + cc) * 128:(half * 4 + cc + 1) * 128], ident[0:QB, 0:QB])
                                       for cc in range(4)], reads=[Rst[k], Rc], writes=[rp_])
                        S.op("act" if half == 0 else "dve",
                             (lambda e, pb=pb, b=b, half=half: e.activation(out=xv[:, half * 4:half * 4 + 4, b * QB:(b + 1) * QB],
                                                                           in_=pb[:, 0:4 * QB].rearrange("p (c t) -> p c t", c=4), func=AF.Copy))
                             if half == 0 else
                             (lambda e, pb=pb, b=b, half=half: e.tensor_copy(xv[:, half * 4:half * 4 + 4, b * QB:(b + 1) * QB],
                                                                            pb[:, 0:4 * QB].rearrange("p (c t) -> p c t", c=4))),
                             reads=[rp_], writes=[Rx])

            def store_tile(dst2d, t0, NT):
                QB = min(128, NT)
                xv = xT[:, :].rearrange("p (c t) -> p c t", c=8)
                for b in range(NT // QB):
                    k = st_rr[0] % 2
                    st_rr[0] += 1
                    for half in range(2):
                        pb, rp_ = bank()
                        S.group("pe", [TR(pb[0:QB, cc * 128:(cc + 1) * 128], xv[:, half * 4 + cc, b * QB:(b + 1) * QB], ident)
                                       for cc in range(4)], reads=[Rx, Rc], writes=[rp_])
                        S.op("act" if half == 0 else "dve",
                             (lambda e, pb=pb, k=k, half=half: e.activation(out=stage[k][0:QB, half * 512:(half + 1) * 512], in_=pb[0:QB, 0:512], func=AF.Copy))
                             if half == 0 else
                             (lambda e, pb=pb, k=k, half=half: e.tensor_copy(stage[k][0:QB, half * 512:(half + 1) * 512], pb[0:QB, 0:512])),
                             reads=[rp_], writes=[Rst[k]])
                    S.dma("sp", "yout0", [(dst2d[t0 + b * QB:t0 + (b + 1) * QB, :], stage[k][0:QB, :])], reads=[Rst[k]])

            for l in range(2):
                S.op("pool", lambda e, l=l: e.memset(kcar[0][l][:, :], 0.0), writes=[res(f"kcar0{l}")])
                S.op("pool", lambda e, l=l: e.memset(vcar[0][l][:, :], 0.0), writes=[res(f"vcar0{l}")])
            S.op("pool", lambda e: e.memset(Hst[0][:, :], 0.0), writes=[res("H0")])
            S.op("pool", lambda e: e.memset(shc[0][:, :], 0.0), writes=[res("shc0")])
            S.op("pool", lambda e: e.memset(ucar[0][:, :], 0.0), writes=[res("ucar0")])
            S.dma("sp", "min", [(Hst[1][:, :], I["h0"][:, :]), (shc[1][:, :], I["sh0"][:, :]), (ucar[1][:, :], I["cv0"][:, :])],
                  writes=[res("H1"), res("shc1"), res("ucar1")])

            ntiles = NPRE + NMAIN
            for ti in range(ntiles):
                pre = ti < NPRE
                partial = pre and (ti < NPRE - 1) and not full_prefix
                last = ti == ntiles - 1
                load_tile(I["xp"], ti * 512, 512)
                fm = "skip" if ti == 0 else ("data" if ti == NPRE else None)
                if NL > 0:
                    attention(0, 0, 512, 0, fm, (O["akp"][0], O["avp"][0], None, None) if last else None)
                if NL > 0.5:
                    mlp(0, 512)
                if NL > 1:
                    rwkv(1, 512, 0, partial, (O["wkvp"], O["shp"]) if last else None)
                if partial:
                    continue
                if NL > 1.5:
                    mlp(1, 512)
                if ti == NPRE:
                    S.op("dve", lambda e: e.tensor_scalar(ucar[0][:, :], ucar[0][:, :], pflag[:, 0:1], None, ALU.mult),
                         reads=[res("ucar0"), res("pflag")], writes=[res("ucar0")])
                if NL > 2:
                    conv(2, 512, 0, O["cvp"] if last else None)
                if NL > 2.5:
                    mlp(2, 512)
                fm3 = "skip" if pre else ("data" if ti == NPRE else None)
                if NL > 3:
                    attention(3, 1, 512, 0, fm3, (O["akp"][1], O["avp"][1], None, None) if last else None)
                if pre:
                    continue
                if NL > 3.5:
                    mlp(3, 512)
                store_tile(O["yp"], (ti - NPRE) * 512, 512)
            phase(sp=True)
            NT = DEC_SEQ
            if 'sample' not in DBG:
                S.finish('sp')
                return
            for l in range(2):
                kst = AR.f32(256)
                Rks = ares(f"kst{l}")
                vst = AR.f32(256)
                Rvs = ares(f"vst{l}")
                S.dma("sp", "min", [(kst[:, :], I["ck"][l]), (vst[:, :], I["cv"][l])], writes=[Rks, Rvs])
                pb, rp_ = bank()
                S.group("pe", [TR(pb[0:64, h * 128:(h + 1) * 128], kst[:, h * 64:(h + 1) * 64], ident) for h in range(4)],
                        reads=[Rks, Rc], writes=[rp_])
                S.op("act", lambda e, pb=pb, l=l: e.activation(out=kcar[1][l][:, :], in_=pb[0:64, 0:512], func=AF.Copy),
                     reads=[rp_], writes=[res(f"kcar1{l}")])
                S.op("dve", lambda e, vst=vst, l=l: e.tensor_copy(vcar[1][l][:, :], vst[:, :]), reads=[Rvs], writes=[res(f"vcar1{l}")])
            load_tile(I["xs"], 0, NT)
            if NL > 0:
                attention(0, 0, NT, 1, None, (O["aks"][0], O["avs"][0], I["ck"][0], I["cv"][0]))
            if NL > 0.5:
                mlp(0, NT)
            if NL > 1:
                rwkv(1, NT, 1, False, (O["wkvs"], O["shs"]))
            if NL > 1.5:
                mlp(1, NT)
            if NL > 2:
                conv(2, NT, 1, O["cvs"])
            if NL > 2.5:
                mlp(2, NT)
            if NL > 3:
                attention(3, 1, NT, 1, None, (O["aks"][1], O["avs"][1], I["ck"][1], I["cv"][1]))
            if NL > 3.5:
                mlp(3, NT)
            store_tile(O["ys"], 0, NT)
            S.finish("sp")

        wsched = []
        emit(Sched(sems, record=True))
        S = Sched(sems, record=False)
        emit(S)
        S.replay(block)
        nops = sum(len(v) for v in S.ops.values())
    return nc, nops


_CACHE = {}


def _cols_layout(vecs):
    out = np.zeros((128, NVEC, 8), np.float32)
    for i, v in enumerate(vecs):
        out[:, i, :] = np.asarray(v, np.float32).reshape(8, 128).T
    return np.ascontiguousarray(out.reshape(128, NVEC * 8))


def kernel(x_prompt, x_sample, cache_a_k, cache_a_v, state_b_wkv, state_b_shift, state_c_conv,
           rel_bias_table, norm_g, a_w_qkv, a_w_o, a_sinks,
           b_mu, b_w_rkv, b_w_o, b_w0, b_w1, b_w2, b_a0, b_a1, b_a2, b_g1, b_g2,
           b_k_k, b_k_a, b_r_k, b_ln_w, b_ln_b,
           c_w_in, c_conv_w, c_w_out, mlp_w1, mlp_w2, _full_prefix=False):
    f = lambda a: np.ascontiguousarray(np.asarray(a, np.float32))
    x_prompt = f(x_prompt)
    B, SEQ, _ = x_prompt.shape
    half = SEQ // 2
    NMAIN = half // 512
    NPRE = NMAIN
    key = (NPRE, NMAIN, _full_prefix)
    if key not in _CACHE:
        _CACHE[key] = build_program(NPRE, NMAIN, _full_prefix)
    nc, _ = _CACHE[key]
    consts = make_consts()
    vecs = [norm_g[i][n] for i in range(4) for n in range(4)] + [b_mu[0][i] for i in range(6)] + [
        b_w0[0], b_a0[0], b_k_k[0], b_k_a[0], np.asarray(b_r_k[0]).reshape(-1), b_ln_w[0], b_ln_b[0],
        c_conv_w[0][0], c_conv_w[0][1], c_conv_w[0][2]]
    cols = _cols_layout(vecs)
    shared = dict(
        consts=consts, cols=cols, table=f(rel_bias_table), sinks=f(a_sinks).reshape(1, 32),
        a_w_qkv=f(a_w_qkv), a_w_o=f(a_w_o), b_w_rkv=f(b_w_rkv[0]), b_w_o=f(b_w_o[0]),
        b_w1=f(b_w1[0]), b_w2=f(b_w2[0]), b_a1=f(b_a1[0]), b_a2=f(b_a2[0]), b_g1=f(b_g1[0]), b_g2=f(b_g2[0]),
        c_w_in=f(c_w_in[0]), c_w_out=f(c_w_out[0]), mlp_w1=f(mlp_w1), mlp_w2=f(mlp_w2),
    )
    cache_a_k = f(cache_a_k)
    cache_a_v = f(cache_a_v)
    state_b_wkv = f(state_b_wkv)
    in_maps = []
    for c in range(8):
        b, hf = c // 2, c % 2
        if hf == 0:
            xp = np.concatenate([np.zeros((half, D), np.float32), x_prompt[b, :half]], axis=0)
            pm = np.full((128, 1), NEG, np.float32)
        else:
            xp = x_prompt[b]
            pm = np.zeros((128, 1), np.float32)
        S0 = state_b_wkv[0, c]
        h0 = S0.reshape(8, 2, 64, 64).transpose(1, 3, 0, 2).reshape(128, 512)
        m = dict(shared)
        m.update(
            xp=np.ascontiguousarray(xp), xs=f(x_sample[c]),
            ck=np.ascontiguousarray(cache_a_k[:, c].reshape(2, 128, 256)),
            cv=np.ascontiguousarray(cache_a_v[:, c].reshape(2, 128, 256)),
            h0=np.ascontiguousarray(h0),
            sh0=np.ascontiguousarray(f(state_b_shift)[0, c].reshape(8, 128).T),
            cv0=np.ascontiguousarray(f(state_c_conv)[0, c].reshape(2, 8, 128).transpose(2, 1, 0).reshape(128, 16)),
            pmask=pm)
        in_maps.append(m)
    res = run_bass_kernel_spmd(nc, in_maps, core_ids=list(range(8))).results

    def unH(a):
        return np.asarray(a).reshape(2, 64, 8, 64).transpose(2, 0, 3, 1).reshape(16, 64, 64)

    def uncol(a):
        return np.asarray(a).T.reshape(1024)

    def uncv(a):
        return np.asarray(a).reshape(128, 8, 2).transpose(2, 1, 0).reshape(2, 1024)

    y_prompt = np.stack([np.concatenate([res[2 * b]["yp"], res[2 * b + 1]["yp"]], axis=0) for b in range(B)]).astype(np.float32)
    y_sample = np.stack([res[c]["ys"] for c in range(8)]).astype(np.float32)
    akp = np.stack([np.stack([res[2 * b + 1]["akp"][l].reshape(128, 4, 64) for b in range(B)]) for l in range(2)]).astype(np.float32)
    avp = np.stack([np.stack([res[2 * b + 1]["avp"][l].reshape(128, 4, 64) for b in range(B)]) for l in range(2)]).astype(np.float32)
    aks = np.stack([np.stack([res[c]["aks"][l].reshape(128, 4, 64) for c in range(8)]) for l in range(2)]).astype(np.float32)
    avs = np.stack([np.stack([res[c]["avs"][l].reshape(128, 4, 64) for c in range(8)]) for l in range(2)]).astype(np.float32)
    wkvp = np.stack([unH(res[2 * b + 1]["wkvp"]) for b in range(B)])[None].astype(np.float32)
    wkvs = np.stack([unH(res[c]["wkvs"]) for c in range(8)])[None].astype(np.float32)
    shp = np.stack([uncol(res[2 * b + 1]["shp"]) for b in range(B)])[None].astype(np.float32)
    shs = np.stack([uncol(res[c]["shs"]) for c in range(8)])[None].astype(np.float32)
    cvp = np.stack([uncv(res[2 * b + 1]["cvp"]) for b in range(B)])[None].astype(np.float32)
    cvs = np.stack([uncv(res[c]["cvs"]) for c in range(8)])[None].astype(np.float32)
    return (y_prompt, y_sample, akp, avp, aks, avs, wkvp, wkvs, shp, shs, cvp, cvs)
```
